# Optimizing a Trainium2 kernel written in Bass

```python
import jax
import jax.numpy as jnp
from jax import lax
import numpy as np

D_MODEL = 1024
BATCH = 16
SEQ = 4096
DEPTH = 1

MEM_LEN = 256
NORM_EPS = 1e-6

MLA_HEADS = 8
MLA_NOPE_DIM = 64
MLA_ROPE_DIM = 32
MLA_QK_DIM = MLA_NOPE_DIM + MLA_ROPE_DIM
MLA_V_DIM = 64
MLA_Q_RANK = 192
MLA_KV_RANK = 128
ROPE_BASE = 10000.0
Q_BLOCK = 128

HG_HEADS = 4
HG_KEY_DIM = 128
HG_VAL_DIM = 128
HG_CHUNK = 64

MLA_WIDTH = MLA_HEADS * MLA_V_DIM
HG_FWIDTH = HG_HEADS * HG_KEY_DIM
HG_WIDTH = HG_HEADS * HG_VAL_DIM
MIX_WIDTH = MLA_WIDTH + HG_WIDTH
IN_SIZES = (MLA_Q_RANK, MLA_KV_RANK, MLA_ROPE_DIM, HG_FWIDTH, HG_FWIDTH, HG_FWIDTH, HG_WIDTH, HG_WIDTH)
IN_WIDTH = sum(IN_SIZES)

X_HEADS = 4
X_HEAD_DIM = D_MODEL // X_HEADS

MOE_GROUPS = 8
MOE_PER_GROUP = 8
MOE_EXPERTS = MOE_GROUPS * MOE_PER_GROUP
MOE_TOP_K = 2
MOE_HIDDEN = 256
MOE_BLOCK = 128

kernel_name = 'hybrid_mla_hgrn2_hmoe_encoder_layer'


def rms_norm(x, w):
    xf = x.astype(jnp.float32)
    y = xf * lax.rsqrt(jnp.mean(xf * xf, axis=-1, keepdims=True) + NORM_EPS)
    return (y * w.astype(jnp.float32)).astype(x.dtype)


def split_columns(p):
    out, start = [], 0
    for size in IN_SIZES:
        out.append(p[..., start:start + size])
        start += size
    return out


def rope_tables(positions):
    half = MLA_ROPE_DIM // 2
    inv_freq = 1.0 / (ROPE_BASE ** (jnp.arange(half, dtype=jnp.float32) / half))
    ang = positions.astype(jnp.float32)[..., None] * inv_freq
    return jnp.cos(ang)[:, :, None, :], jnp.sin(ang)[:, :, None, :]


def apply_rope(x, cos, sin):
    x1, x2 = jnp.split(x.astype(jnp.float32), 2, axis=-1)
    return jnp.concatenate([x1 * cos - x2 * sin, x2 * cos + x1 * sin], axis=-1).astype(x.dtype)


def blocked_attention(q, k, v, scale):
    B, S, H, Dq = q.shape
    nb = S // Q_BLOCK
    qb = q.reshape(B, nb, Q_BLOCK, H, Dq).transpose(1, 0, 2, 3, 4)

    def one_block(qblk):
        s = jnp.einsum('bqhd,bkhd->bhqk', qblk, k).astype(jnp.float32) * scale
        p = jax.nn.softmax(s, axis=-1).astype(v.dtype)
        return jnp.einsum('bhqk,bkhd->bqhd', p, v)

    o = lax.map(one_block, qb)
    return o.transpose(1, 0, 2, 3, 4).reshape(B, S, H, v.shape[-1])


def mla_mixer(c_q, c_kv, k_rope, cos, sin, q_a_norm, w_q_up, kv_a_norm, w_kv_up, q_norm, k_norm):
    B, S, _ = c_q.shape
    q = (rms_norm(c_q, q_a_norm) @ w_q_up).reshape(B, S, MLA_HEADS, MLA_QK_DIM)
    kv = (rms_norm(c_kv, kv_a_norm) @ w_kv_up).reshape(B, S, MLA_HEADS, MLA_NOPE_DIM + MLA_V_DIM)
    k_nope, v = kv[..., :MLA_NOPE_DIM], kv[..., MLA_NOPE_DIM:]
    k_r = jnp.broadcast_to(k_rope[:, :, None, :], (B, S, MLA_HEADS, MLA_ROPE_DIM))
    k = jnp.concatenate([k_nope, k_r], axis=-1)
    q = rms_norm(q, q_norm)
    k = rms_norm(k, k_norm)
    q = jnp.concatenate([q[..., :MLA_NOPE_DIM], apply_rope(q[..., MLA_NOPE_DIM:], cos, sin)], axis=-1)
    k = jnp.concatenate([k[..., :MLA_NOPE_DIM], apply_rope(k[..., MLA_NOPE_DIM:], cos, sin)], axis=-1)
    o = blocked_attention(q, k, v, MLA_QK_DIM ** -0.5)
    return o.reshape(B, S, MLA_WIDTH)


def gla_chunk_scan(q, k, g, v):
    B, S, H, DK = q.shape
    DV = v.shape[-1]
    n = S // HG_CHUNK

    def to_chunks(t):
        return t.astype(jnp.float32).reshape(B, n, HG_CHUNK, H, t.shape[-1]).transpose(1, 0, 3, 2, 4)

    qc, kc, gc, vc = to_chunks(q), to_chunks(k), to_chunks(g), to_chunks(v)
    mask = jnp.tril(jnp.ones((HG_CHUNK, HG_CHUNK), dtype=bool))

    def step(state, inp):
        qi, ki, gi, vi = inp
        b = jnp.cumsum(gi, axis=2)
        o_inter = jnp.einsum('bhtd,bhdv->bhtv', qi * jnp.exp(b), state)
        diff = b[:, :, :, None, :] - b[:, :, None, :, :]
        decay = jnp.exp(jnp.where(mask[:, :, None], diff, -jnp.inf))
        attn = jnp.einsum('bhtd,bhsd,bhtsd->bhts', qi, ki, decay)
        o_intra = jnp.einsum('bhts,bhsv->bhtv', attn, vi)
        b_last = b[:, :, -1:, :]
        new_state = jnp.exp(b_last[:, :, 0, :])[..., None] * state + jnp.einsum(
            'bhsd,bhsv->bhdv', ki * jnp.exp(b_last - b), vi)
        return new_state, o_inter + o_intra

    s0 = jnp.zeros((B, H, DK, DV), jnp.float32)
    _, o = lax.scan(step, s0, (qc, kc, gc, vc))
    return o.transpose(1, 0, 3, 2, 4).reshape(B, S, H, DV)


def hgrn2_mixer(hq, hf_fwd, hf_bwd, hi, hg, lb, o_norm):
    B, S, _ = hq.shape
    q = jax.nn.silu(hq).reshape(B, S, HG_HEADS, HG_KEY_DIM)
    i = hi.reshape(B, S, HG_HEADS, HG_VAL_DIM)

    def direction(f_pre, lb_dir, reverse):
        z = f_pre.astype(jnp.float32)
        log_f = jnp.logaddexp(jnp.log(lb_dir), jnp.log1p(-lb_dir) + jax.nn.log_sigmoid(z))
        log_f = log_f.reshape(B, S, HG_HEADS, HG_KEY_DIM)
        k = -jnp.expm1(log_f)
        args = (q, k, log_f, i)
        if reverse:
            args = tuple(jnp.flip(a, axis=1) for a in args)
        o = gla_chunk_scan(*args)
        return jnp.flip(o, axis=1) if reverse else o

    o = direction(hf_fwd, lb[0], False) + direction(hf_bwd, lb[1], True)
    o = rms_norm(o, o_norm).astype(hq.dtype) * jax.nn.silu(hg.reshape(B, S, HG_HEADS, HG_VAL_DIM))
    return o.reshape(B, S, HG_WIDTH)


def cross_attention(h, m, w_q, w_kv, q_norm, k_norm, w_o):
    B, S, D = h.shape
    M = m.shape[1]
    q = rms_norm((h @ w_q).reshape(B, S, X_HEADS, X_HEAD_DIM), q_norm)
    kv = (m @ w_kv).reshape(B, M, 2, X_HEADS, X_HEAD_DIM)
    k = rms_norm(kv[:, :, 0], k_norm)
    v = kv[:, :, 1]
    s = jnp.einsum('bqhd,bkhd->bhqk', q, k).astype(jnp.float32) * (X_HEAD_DIM ** -0.5)
    p = jax.nn.softmax(s, axis=-1).astype(v.dtype)
    o = jnp.einsum('bhqk,bkhd->bqhd', p, v).reshape(B, S, X_HEADS * X_HEAD_DIM)
    return o @ w_o


def dropless_experts(t, expert_id, weight, w_gate, w_up, w_down):
    T, D = t.shape
    A = T * MOE_TOP_K
    e_flat = expert_id.reshape(A)
    w_flat = weight.reshape(A)
    tok_flat = jnp.arange(A, dtype=jnp.int32) // MOE_TOP_K
    order = jnp.argsort(e_flat)
    e_sorted, tok_sorted, w_sorted = e_flat[order], tok_flat[order], w_flat[order]
    counts = jnp.bincount(e_flat, length=MOE_EXPERTS)
    padded = (counts + MOE_BLOCK - 1) // MOE_BLOCK * MOE_BLOCK
    start = jnp.cumsum(counts) - counts
    pend = jnp.cumsum(padded)
    pstart = pend - padded
    dest = pstart[e_sorted] + jnp.arange(A, dtype=jnp.int32) - start[e_sorted]
    n_blocks = -(-A // MOE_BLOCK) + MOE_EXPERTS
    rows = jnp.zeros((n_blocks * MOE_BLOCK, D), t.dtype).at[dest].set(t[tok_sorted])
    block_e = jnp.searchsorted(pend, jnp.arange(n_blocks, dtype=jnp.int32) * MOE_BLOCK, side='right')
    block_e = jnp.minimum(block_e, MOE_EXPERTS - 1)

    def expert_block(args):
        xb, e = args
        hb = jax.nn.silu(xb @ w_gate[e]) * (xb @ w_up[e])
        return hb @ w_down[e]

    out = lax.map(expert_block, (rows.reshape(n_blocks, MOE_BLOCK, D), block_e)).reshape(-1, D)
    contrib = out[dest] * w_sorted[:, None].astype(out.dtype)
    return jnp.zeros((T, D), out.dtype).at[tok_sorted].add(contrib)


def hier_moe(h, w_group, b_group, w_expert, b_expert, w_gate, w_up, w_down):
    B, S, D = h.shape
    t = h.reshape(B * S, D)
    g_logits = (t @ w_group).astype(jnp.float32) + b_group.astype(jnp.float32)
    g_prob = jax.nn.softmax(g_logits, axis=-1)
    g_sel = jnp.argmax(g_logits, axis=-1).astype(jnp.int32)
    g_weight = jnp.take_along_axis(g_prob, g_sel[:, None], axis=-1)
    e_logits = ((t @ w_expert).astype(jnp.float32) + b_expert.astype(jnp.float32)).reshape(
        -1, MOE_GROUPS, MOE_PER_GROUP)
    e_logits = jnp.take_along_axis(e_logits, g_sel[:, None, None], axis=1)[:, 0]
    top_logit, top_local = lax.top_k(e_logits, MOE_TOP_K)
    top_w = jax.nn.softmax(top_logit, axis=-1) * g_weight
    expert_id = g_sel[:, None] * MOE_PER_GROUP + top_local.astype(jnp.int32)
    y = dropless_experts(t, expert_id, top_w, w_gate, w_up, w_down)
    return y.reshape(B, S, D)


def setup_inputs(seed: int = 0) -> dict:
    key = jax.random.key(seed)
    keys = jax.random.split(key, 40)
    counter = [0]

    def nk():
        k = keys[counter[0]]
        counter[0] += 1
        return k

    def nrm(shape, fan_in):
        return jax.random.normal(nk(), shape, jnp.float32) * (fan_in ** -0.5)

    def gain(shape):
        return 1.0 + 0.02 * jax.random.normal(nk(), shape, jnp.float32)

    L = DEPTH
    x = jax.random.normal(nk(), (BATCH, SEQ, D_MODEL), jnp.float32)
    mem = jax.random.normal(nk(), (BATCH, MEM_LEN, D_MODEL), jnp.float32)
    positions = jnp.arange(SEQ, dtype=jnp.int32)[None, :] + jax.random.randint(
        nk(), (BATCH, 1), 0, SEQ, dtype=jnp.int32)
    return {
        'x': x,
        'mem': mem,
        'positions': positions,
        'norm_mix': gain((L, D_MODEL)),
        'w_in': nrm((L, D_MODEL, IN_WIDTH), D_MODEL),
        'mla_q_a_norm': gain((L, MLA_Q_RANK)),
        'mla_w_q_up': nrm((L, MLA_Q_RANK, MLA_HEADS * MLA_QK_DIM), MLA_Q_RANK),
        'mla_kv_a_norm': gain((L, MLA_KV_RANK)),
        'mla_w_kv_up': nrm((L, MLA_KV_RANK, MLA_HEADS * (MLA_NOPE_DIM + MLA_V_DIM)), MLA_KV_RANK),
        'mla_q_norm': gain((L, MLA_QK_DIM)),
        'mla_k_norm': gain((L, MLA_QK_DIM)),
        'hg_lb_logits': 0.1 * jax.random.normal(nk(), (L + 1, 2, HG_FWIDTH), jnp.float32),
        'hg_o_norm': gain((L, HG_VAL_DIM)),
        'w_out': nrm((L, MIX_WIDTH, D_MODEL), MIX_WIDTH),
        'norm_cross': gain((L, D_MODEL)),
        'norm_mem': gain((L, D_MODEL)),
        'x_w_q': nrm((L, D_MODEL, X_HEADS * X_HEAD_DIM), D_MODEL),
        'x_w_kv': nrm((L, D_MODEL, 2 * X_HEADS * X_HEAD_DIM), D_MODEL),
        'x_q_norm': gain((L, X_HEAD_DIM)),
        'x_k_norm': gain((L, X_HEAD_DIM)),
        'x_w_o': nrm((L, X_HEADS * X_HEAD_DIM, D_MODEL), X_HEADS * X_HEAD_DIM),
        'norm_ffn': gain((L, D_MODEL)),
        'moe_w_group': nrm((L, D_MODEL, MOE_GROUPS), D_MODEL),
        'moe_b_group': 0.01 * jax.random.normal(nk(), (L, MOE_GROUPS), jnp.float32),
        'moe_w_expert': nrm((L, D_MODEL, MOE_EXPERTS), D_MODEL),
        'moe_b_expert': 0.01 * jax.random.normal(nk(), (L, MOE_EXPERTS), jnp.float32),
        'moe_w_gate': nrm((L, MOE_EXPERTS, D_MODEL, MOE_HIDDEN), D_MODEL),
        'moe_w_up': nrm((L, MOE_EXPERTS, D_MODEL, MOE_HIDDEN), D_MODEL),
        'moe_w_down': nrm((L, MOE_EXPERTS, MOE_HIDDEN, D_MODEL), MOE_HIDDEN),
    }


def reference(x, mem, positions, norm_mix, w_in, mla_q_a_norm, mla_w_q_up, mla_kv_a_norm, mla_w_kv_up,
              mla_q_norm, mla_k_norm, hg_lb_logits, hg_o_norm, w_out, norm_cross, norm_mem, x_w_q, x_w_kv,
              x_q_norm, x_k_norm, x_w_o, norm_ffn, moe_w_group, moe_b_group, moe_w_expert, moe_b_expert,
              moe_w_gate, moe_w_up, moe_w_down):
    cos, sin = rope_tables(positions)
    lb_table = jnp.cumsum(jax.nn.softmax(hg_lb_logits.astype(jnp.float32), axis=0), axis=0)
    for l in range(DEPTH):
        h = rms_norm(x, norm_mix[l])
        c_q, c_kv, k_rope, hq, hf_fwd, hf_bwd, hi, hg = split_columns(h @ w_in[l])
        a = mla_mixer(c_q, c_kv, k_rope, cos, sin, mla_q_a_norm[l], mla_w_q_up[l], mla_kv_a_norm[l],
                      mla_w_kv_up[l], mla_q_norm[l], mla_k_norm[l])
        r = hgrn2_mixer(hq, hf_fwd, hf_bwd, hi, hg, lb_table[l], hg_o_norm[l])
        x = x + jnp.concatenate([a, r], axis=-1) @ w_out[l]
        x = x + cross_attention(rms_norm(x, norm_cross[l]), rms_norm(mem, norm_mem[l]), x_w_q[l], x_w_kv[l],
                                x_q_norm[l], x_k_norm[l], x_w_o[l])
        x = x + hier_moe(rms_norm(x, norm_ffn[l]), moe_w_group[l], moe_b_group[l], moe_w_expert[l],
                         moe_b_expert[l], moe_w_gate[l], moe_w_up[l], moe_w_down[l])
    return x
```

```python
import contextlib
import math
import numpy as np
import concourse.bass as bass
import concourse.mybir as mybir
from concourse.bass_utils import run_bass_kernel_spmd

F32 = mybir.dt.float32
BF16 = mybir.dt.bfloat16
I32 = mybir.dt.int32
U32 = mybir.dt.uint32
AF = mybir.ActivationFunctionType
ALU = mybir.AluOpType
AX = mybir.AxisListType

D = 1024
EPS = 1e-6
NEXP = 64
IN_W = 2912


class Sched:
    ENGS = ("pe", "dve", "act", "pool", "sp")

    def __init__(self, nc, self_sync=False):
        self.nc = nc
        self.self_sync = self_sync
        self.stream = {e: [] for e in self.ENGS}
        self.sem_count = {}
        self.known = {e: {} for e in self.ENGS}
        self.last_w = {}
        self.readers = {}
        self.dma_last = {}
        self.sems = {}
        self.stack = contextlib.ExitStack()
        self.nops = 0

    def _sem(self, name):
        if name not in self.sems:
            self.sems[name] = self.stack.enter_context(self.nc.semaphore(name))
        return self.sems[name]

    def _emit(self, eng, fn, reads, writes, sem, inc, extra=()):
        deps = list(extra)
        for r in reads:
            w = self.last_w.get(r)
            if w is not None:
                deps.append(w)
        for r in writes:
            w = self.last_w.get(r)
            if w is not None:
                deps.append(w)
            deps.extend(self.readers.get(r, ()))
        known = self.known[eng]
        waits = {}
        for (s, v, vc) in deps:
            if s == eng and (eng == "pe" or not self.self_sync):
                continue
            if known.get(s, 0) >= v:
                continue
            waits[s] = max(waits.get(s, 0), v)
            for k2, v2 in vc.items():
                if known.get(k2, 0) < v2:
                    known[k2] = v2
            known[s] = max(known.get(s, 0), v)
        val = self.sem_count.get(sem, 0) + inc
        self.sem_count[sem] = val
        vc = dict(known)
        vc[sem] = val
        me = (sem, val, vc)
        for r in reads:
            self.readers.setdefault(r, []).append(me)
        for r in writes:
            self.last_w[r] = me
            self.readers[r] = []
        self._sem(sem)
        self.stream[eng].append((sorted(waits.items()), fn, sem, inc))
        self.nops += 1
        return me

    def op(self, eng, fn, reads=(), writes=()):
        return self._emit(eng, fn, tuple(reads), tuple(writes), eng, 1)

    def dma(self, queue, key, fn, reads=(), writes=()):
        sem = "d_" + key
        extra = [self.dma_last[sem]] if sem in self.dma_last else []
        me = self._emit(queue, fn, tuple(reads), tuple(writes), sem, 16, extra)
        self.dma_last[sem] = me
        return me

    def flush(self, barrier=True):
        nc = self.nc
        if barrier:
            waits = sorted(self.sem_count.items())
            for e in self.ENGS:
                self.stream[e].append((waits, None, None, 0))
                for s, v in waits:
                    self.known[e][s] = max(self.known[e].get(s, 0), v)
        streams = self.stream
        self.stream = {e: [] for e in self.ENGS}
        sems = self.sems
        with nc.Block() as block:
            def replay(name):
                def body(e):
                    for waits, fn, sem, inc in streams[name]:
                        for s, v in waits:
                            e.wait_ge(sems[s], v)
                        if fn is not None:
                            fn(e).then_inc(sems[sem], inc)
                return body
            block.tensor(replay("pe"))
            block.vector(replay("dve"))
            block.scalar(replay("act"))
            block.gpsimd(replay("pool"))
            block.sync(replay("sp"))


def build(S, CAP, dbg=False):
    NB = 2
    T = NB * S
    NT = S // 128
    QC = min(512, S)
    NQC = S // QC
    BLK = min(512, S)
    CT = CAP // 128
    nc = bass.Bass("TRN2", target_bir_lowering=False)

    def din(name, shape, dt=F32):
        return nc.dram_tensor(name, list(shape), dt, kind="ExternalInput").ap()

    def dscr(name, shape, dt):
        return nc.dram_tensor(name, list(shape), dt, kind=("ExternalOutput" if dbg else "Internal")).ap()

    x_d = din("x", [NB, S, D]); mem_d = din("mem", [NB, 256, D]); pos_d = din("positions", [NB, S], I32)
    invf_d = din("inv_freq", [1, 16])
    norm_mix = din("norm_mix", [1, D]); w_in = din("w_in", [D, IN_W])
    q_a_norm = din("mla_q_a_norm", [1, 192]); w_q_up = din("mla_w_q_up", [192, 768])
    kv_a_norm = din("mla_kv_a_norm", [1, 128]); w_kv_up = din("mla_w_kv_up", [128, 1024])
    q_norm = din("mla_q_norm", [1, 96]); k_norm = din("mla_k_norm", [1, 96])
    lb_logits = din("hg_lb_logits", [2, 8, 128]); o_norm = din("hg_o_norm", [1, 128])
    w_out = din("w_out", [D, D]); norm_cross = din("norm_cross", [1, D]); norm_mem = din("norm_mem", [1, D])
    x_w_q = din("x_w_q", [D, D]); x_w_kv = din("x_w_kv", [D, 2 * D])
    x_q_norm = din("x_q_norm", [1, 256]); x_k_norm = din("x_k_norm", [1, 256]); x_w_o = din("x_w_o", [D, D])
    norm_ffn = din("norm_ffn", [1, D]); w_rt = din("moe_w_router", [D, 72]); b_rt = din("moe_b_router", [1, 72])
    w_gate = din("moe_w_gate", [NEXP, D, 256]); w_up = din("moe_w_up", [NEXP, D, 256]); w_down = din("moe_w_down", [NEXP, 256, D])
    y_d = nc.dram_tensor("y", [T, D], F32, kind="ExternalOutput").ap()

    qT_d = dscr("qT", [NB, 8, 96, S], BF16); kT_d = dscr("kT", [NB, 8, 96, S], BF16)
    vE_d = dscr("vE", [NB, 8, S, 128], BF16); pT_d = dscr("pT", [NB, 12, 128, S], F32)
    hi_d = dscr("hi", [NB, S, 512], BF16); sg_d = dscr("sg", [NB, S, 512], BF16)
    x2_d = dscr("x2", [T, D], F32); h3_d = dscr("h3", [T, D], BF16)
    aT_d = dscr("aTd", [NB, 4, 128, S], BF16)
    Xs_d = dscr("Xs", [NEXP * CAP, D], BF16); Yb_d = dscr("Yb", [NEXP * CAP, D], BF16)
    wgub_d = nc.dram_tensor("wgub", [NEXP, 128, 8 * 512], BF16, kind="Internal").ap()
    wdb_d = nc.dram_tensor("wdb", [NEXP, 128, 2 * D], BF16, kind="Internal").ap()
    wcb_d = nc.dram_tensor("wcb", [3, 128, 8 * D], BF16, kind="Internal").ap()
    winb_d = nc.dram_tensor("winb", [128, 8 * IN_W], BF16, kind="Internal").ap()

    if dbg:
        dx1_d = dscr("dbg_x1", [T, D], F32); dr_d = dscr("dbg_r", [T, 512], BF16)
    Sx = Sched(nc, self_sync=True)
    cur = [Sx]

    def op(*a, **k):
        return cur[0].op(*a, **k)

    def dma(*a, **k):
        return cur[0].dma(*a, **k)

    class Rec:
        def __init__(self):
            self.items = []

        def op(self, *a, **k):
            self.items.append((0, a, k))

        def dma(self, *a, **k):
            self.items.append((1, a, k))

    def record(fn, *args):
        r = Rec()
        cur[0] = r
        try:
            fn(*args)
        finally:
            cur[0] = Sx
        return r.items

    def emit_zip(*lists):
        lists = [l for l in lists if l]
        idx = [0] * len(lists)
        while True:
            best = None
            for q_, l in enumerate(lists):
                if idx[q_] < len(l):
                    pr_ = idx[q_] / len(l)
                    if best is None or pr_ < best[0]:
                        best = (pr_, q_)
            if best is None:
                break
            q_ = best[1]
            it_ = lists[q_][idx[q_]]
            idx[q_] += 1
            (Sx.dma if it_[0] else Sx.op)(*it_[1], **it_[2])
    top = contextlib.ExitStack()

    RN = {}
    uid = [0]

    def alloc(st, name, shape, dt=F32):
        uid[0] += 1
        h = st.enter_context(nc.sbuf_tensor("%s_u%d" % (name, uid[0]), list(shape), dt))
        RN[h.name] = name
        return h

    def palloc(st, name, shape, dt=F32):
        uid[0] += 1
        h = st.enter_context(nc.psum_tensor("%s_u%d" % (name, uid[0]), list(shape), dt))
        RN[h.name] = name
        return h

    def rn(t):
        return RN[t.name]

    def bcast_load(t, vec, n, key):
        dma("sp", key, lambda e: e.dma_start(out=t[:, 0:n], in_=vec.partition_broadcast(128)), writes=[rn(t)])

    identb = alloc(top, "identb", [128, 128], BF16); identf = alloc(top, "identf", [128, 128])
    maskf = alloc(top, "maskf", [128, 128]); maskb = alloc(top, "maskb", [128, 128])
    ltri = alloc(top, "ltri", [128, 128], BF16); onesb = alloc(top, "onesb", [128, 128], BF16)
    g_cross = alloc(top, "g_cross", [128, D]); g_ffn = alloc(top, "g_ffn", [128, D])
    g_o = alloc(top, "g_o", [128, 4, 128]); g_xq = alloc(top, "g_xq", [128, 4, 256])
    b_r = alloc(top, "b_r", [128, 72]); ecap = alloc(top, "ecap", [128, 64])
    lbT = alloc(top, "lbT", [128, 8]); omlT = alloc(top, "omlT", [128, 8]); hoT = alloc(top, "hoT", [128, 8]); lbhT = alloc(top, "lbhT", [128, 8])
    xKT = alloc(top, "xKT", [128, NB, 8, 256], BF16); xV = alloc(top, "xV", [128, NB, 2, D], BF16)
    rt = alloc(top, "rt", [128, NB * NT, 4])
    rti = alloc(top, "rti", [128, NB * NT, 2], I32)
    cnt = alloc(top, "cnt", [128, 64])
    wrt = alloc(top, "wrt", [128, 8, 72])
    tmpc = alloc(top, "tmpc", [128, 256])
    epsb = alloc(top, "epsb", [128, 1])

    with contextlib.ExitStack() as st:
        ptr0 = palloc(st, "ptr0", [128, 512])
        op("pool", lambda e: e.memset(identf[:], 1.0), writes=["identf"])
        op("pool", lambda e: e.affine_select(identf[:], identf[:], [[-1, 128]], ALU.is_equal, 0.0, base=0, channel_multiplier=1),
           reads=["identf"], writes=["identf"])
        op("dve", lambda e: e.tensor_copy(identb[:], identf[:]), reads=["identf"], writes=["identb"])
        op("pool", lambda e: e.memset(maskf[:], 1.0), writes=["maskf"])
        op("pool", lambda e: e.affine_select(maskf[:], maskf[:], [[1, 128]], ALU.is_ge, 0.0, base=0, channel_multiplier=-1),
           reads=["maskf"], writes=["maskf"])
        op("pool", lambda e: e.memset(maskb[:], 1.0), writes=["maskb"])
        op("pool", lambda e: e.affine_select(maskb[:], maskb[:], [[-1, 128]], ALU.is_ge, 0.0, base=0, channel_multiplier=1),
           reads=["maskb"], writes=["maskb"])
        op("pool", lambda e: e.memset(tmpc[:, 0:128], 1.0), writes=["tmpc"])
        op("pool", lambda e: e.affine_select(tmpc[:, 0:128], tmpc[:, 0:128], [[1, 128]], ALU.is_gt, 0.0, base=0, channel_multiplier=-1),
           reads=["tmpc"], writes=["tmpc"])
        op("dve", lambda e: e.tensor_copy(ltri[:], tmpc[:, 0:128]), reads=["tmpc"], writes=["ltri"])
        op("dve", lambda e: e.memset(onesb[:], 1.0), writes=["onesb"])
        op("dve", lambda e: e.memset(epsb[:], EPS), writes=["epsb"])
        op("dve", lambda e: e.memset(cnt[:], 0.0), writes=["cnt"])
        op("pool", lambda e: e.iota(ecap[:], [[CAP, 64]], base=0, channel_multiplier=0, allow_small_or_imprecise_dtypes=True), writes=["ecap"])
        bcast_load(g_cross, norm_cross, D, "c1"); bcast_load(g_ffn, norm_ffn, D, "c2")
        bcast_load(b_r, b_rt, 72, "c1")
        for h in range(4):
            dma("sp", "c0", lambda e, h=h: e.dma_start(out=g_o[:, h, :], in_=o_norm.partition_broadcast(128)), writes=["g_o"])
            dma("sp", "c1", lambda e, h=h: e.dma_start(out=g_xq[:, h, :], in_=x_q_norm.partition_broadcast(128)), writes=["g_xq"])
        op("dve", lambda e: e.tensor_scalar(g_xq[:], g_xq[:], 256.0 ** -0.5, None, ALU.mult), reads=["g_xq"], writes=["g_xq"])
        dma("sp", "c2", lambda e: e.dma_start(out=wrt[:], in_=w_rt.rearrange("(c p) n -> p c n", p=128)), writes=["wrt"])
        dma("sp", "c3", lambda e: e.dma_start(out=tmpc[0:8, 0:128], in_=lb_logits[0]), writes=["tmpc"])
        dma("sp", "c0", lambda e: e.dma_start(out=tmpc[0:8, 128:256], in_=lb_logits[1]), writes=["tmpc"])
        op("dve", lambda e: e.tensor_tensor(tmpc[0:8, 0:128], tmpc[0:8, 0:128], tmpc[0:8, 128:256], ALU.subtract), reads=["tmpc"], writes=["tmpc"])
        op("act", lambda e: e.activation(out=tmpc[0:8, 0:128], in_=tmpc[0:8, 0:128], func=AF.Sigmoid), reads=["tmpc"], writes=["tmpc"])
        op("pe", lambda e: e.transpose(ptr0[:, 0:8], tmpc[0:8, 0:128], identf[0:8, 0:8]), reads=["tmpc", "identf"], writes=["ptr0"])
        op("dve", lambda e: e.tensor_copy(lbT[:], ptr0[:, 0:8]), reads=["ptr0"], writes=["lbT"])
        op("dve", lambda e: e.tensor_scalar(omlT[:], lbT[:], -1.0, 1.0, ALU.mult, ALU.add), reads=["lbT"], writes=["omlT"])
        op("dve", lambda e: e.tensor_scalar(hoT[:], omlT[:], 0.5, None, ALU.mult), reads=["omlT"], writes=["hoT"])
        op("dve", lambda e: e.tensor_tensor(lbhT[:], lbT[:], hoT[:], ALU.add), reads=["lbT", "hoT"], writes=["lbhT"])
        Sx.flush()
    if dbg == "0":
        return nc

    def rstd_from_ms(ms, n, tag):
        op("act", lambda e: e.activation(out=ms[:, 0:n], in_=ms[:, 0:n], func=AF.Ln, bias=epsb[:, 0:1]), reads=[tag, "epsb"], writes=[tag])
        op("act", lambda e: e.activation(out=ms[:, 0:n], in_=ms[:, 0:n], func=AF.Exp, scale=-0.5), reads=[tag], writes=[tag])

    with contextlib.ExitStack() as st:
        wkv = alloc(st, "wkv", [128, 8, 2 * D], BF16)
        g_mem = alloc(st, "g_mem", [128, D]); g_xk = alloc(st, "g_xk", [128, 4, 256])
        mt = alloc(st, "mt", [128, D]); mb = alloc(st, "mb", [128, D], BF16); mT = alloc(st, "mT", [128, 8, 128], BF16)
        junk = alloc(st, "junkM", [128, D]); ms = alloc(st, "msM", [128, 8])
        kf = alloc(st, "kfM", [128, 4, 256]); kb = alloc(st, "kbM", [128, 4, 256], BF16)
        ptr = palloc(st, "ptrM", [128, 1024], BF16)
        pkv = [palloc(st, "pkv%d" % i, [128, 512]) for i in range(4)]
        bcast_load(g_mem, norm_mem, D, "c0")
        for h in range(4):
            dma("sp", "c1", lambda e, h=h: e.dma_start(out=g_xk[:, h, :], in_=x_k_norm.partition_broadcast(128)), writes=["g_xk"])
        for kc in range(8):
            dma("pool", "w%d" % (kc % 2), lambda e, kc=kc: e.dma_start(out=wkv[:, kc, :], in_=x_w_kv[kc * 128:(kc + 1) * 128, :]), writes=["wkv"])
        for b in range(NB):
            for mtile in range(2):
                dma("sp", "mx", lambda e, b=b, mtile=mtile: e.dma_start(out=mt[:], in_=mem_d[b, mtile * 128:(mtile + 1) * 128, :]), writes=["mt"])
                op("act", lambda e: e.activation(out=junk[:], in_=mt[:], func=AF.Square, scale=1.0 / 32.0),
                   reads=["mt"], writes=["junkM"])
                op("dve", lambda e: e.tensor_reduce(ms[:, 0:1], junk[:], AX.X, ALU.add), reads=["junkM"], writes=["msM"])
                rstd_from_ms(ms, 1, "msM")
                op("dve", lambda e: e.scalar_tensor_tensor(mb[:], mt[:], ms[:, 0:1], g_mem[:], ALU.mult, ALU.mult), reads=["mt", "msM", "g_mem"], writes=["mb"])
                for kc in range(8):
                    op("pe", lambda e, kc=kc: e.transpose(ptr[:, kc * 128:(kc + 1) * 128], mb[:, kc * 128:(kc + 1) * 128], identb[:]),
                       reads=["mb", "identb"], writes=["ptrM"])
                op("act", lambda e: e.activation(out=mT[:].rearrange("p a b -> p (a b)"), in_=ptr[:], func=AF.Copy), reads=["ptrM"], writes=["mT"])
                for g in range(4):
                    for kc in range(8):
                        op("pe", lambda e, g=g, kc=kc: e.matmul(pkv[g][:], mT[:, kc, :], wkv[:, kc, g * 512:(g + 1) * 512], start=(kc == 0), stop=(kc == 7)),
                           reads=["mT", "wkv"], writes=["pkv%d" % g])
                for g in range(2):
                    op("act", lambda e, g=g: e.activation(out=kf[:, 2 * g:2 * g + 2, :].rearrange("p a b -> p (a b)"), in_=pkv[g][:], func=AF.Copy),
                       reads=["pkv%d" % g], writes=["kfM"])
                    op("dve", lambda e, g=g, b=b, mtile=mtile: e.tensor_copy(xV[:, b, mtile, g * 512:(g + 1) * 512], pkv[2 + g][:]),
                       reads=["pkv%d" % (2 + g)], writes=["xV"])
                for h in range(4):
                    op("act", lambda e, h=h: e.activation(out=junk[:, 0:256], in_=kf[:, h, :], func=AF.Square, scale=1.0 / 16.0),
                       reads=["kfM"], writes=["junkM"])
                    op("dve", lambda e, h=h: e.tensor_reduce(ms[:, h:h + 1], junk[:, 0:256], AX.X, ALU.add), reads=["junkM"], writes=["msM"])
                rstd_from_ms(ms, 4, "msM")
                op("dve", lambda e: e.tensor_tensor(kf[:], kf[:], ms[:, 0:4].unsqueeze(2).to_broadcast([128, 4, 256]), ALU.mult), reads=["kfM", "msM"], writes=["kfM"])
                op("dve", lambda e: e.tensor_tensor(kb[:], kf[:], g_xk[:], ALU.mult), reads=["kfM", "g_xk"], writes=["kbM"])
                for c in range(8):
                    op("pe", lambda e, c=c: e.transpose(ptr[:, c * 128:(c + 1) * 128], kb[:, c // 2, (c % 2) * 128:(c % 2 + 1) * 128], identb[:]),
                       reads=["kbM", "identb"], writes=["ptrM"])
                op("act", lambda e, b=b, mtile=mtile: e.activation(out=xKT[:, b, :, mtile * 128:(mtile + 1) * 128],
                                                                     in_=ptr[:].rearrange("p (a b) -> p a b", a=8), func=AF.Copy),
                   reads=["ptrM"], writes=["xKT"])
        Sx.flush()
    if dbg == "M":
        return nc

    aT_stack = contextlib.ExitStack()
    for b in range(NB):
        with contextlib.ExitStack() as st:
            win = alloc(st, "win", [128, 8, IN_W], BF16)
            g_mix = alloc(st, "g_mix", [128, D]); g_qa = alloc(st, "g_qa", [128, 192]); g_kva = alloc(st, "g_kva", [128, 128])
            gq96 = alloc(st, "gq96", [128, 8, 96]); gk96 = alloc(st, "gk96", [128, 8, 96])
            bcast_load(g_mix, norm_mix, D, "c0"); bcast_load(g_qa, q_a_norm, 192, "c3"); bcast_load(g_kva, kv_a_norm, 128, "c0")
            for h in range(8):
                dma("sp", "c2", lambda e, h=h: e.dma_start(out=gq96[:, h, :], in_=q_norm.partition_broadcast(128)), writes=["gq96"])
                dma("sp", "c3", lambda e, h=h: e.dma_start(out=gk96[:, h, :], in_=k_norm.partition_broadcast(128)), writes=["gk96"])
            op("dve", lambda e: e.tensor_scalar(gq96[:], gq96[:], 96.0 ** -0.5, None, ALU.mult), reads=["gq96"], writes=["gq96"])
            wq = alloc(st, "wq", [128, 2, 768], BF16); wkvu = alloc(st, "wkvu", [128, 1024], BF16)
            cs = alloc(st, "cs", [128, NT, 2, 16]); ang = alloc(st, "ang", [128, NT, 2, 16]); kk = alloc(st, "kk", [128, NT, 2, 16])
            posi = alloc(st, "posi", [NT, 128], I32); posf = alloc(st, "posf", [NT, 128]); posT = alloc(st, "posT", [128, NT])
            invf = alloc(st, "invf", [128, 16])
            xt = [alloc(st, "xt%d" % i, [128, D]) for i in range(2)]
            junk = alloc(st, "junkA", [128, D])
            hb = alloc(st, "hb", [128, D], BF16); hT4s = [alloc(st, "hT4_%d" % i, [128, 8, 512], BF16) for i in range(2)]
            ms = alloc(st, "msA", [128, 4]); msh = alloc(st, "msh", [128, 48])
            cn = alloc(st, "cn", [128, 320], BF16); cnTs = [alloc(st, "cnT%d" % i, [128, 3, 128], BF16) for i in range(2)]; krs = alloc(st, "krs", [128, 32])
            qf = alloc(st, "qf", [128, 8, 96]); kfs = [alloc(st, "kf%d" % i, [128, 8, 96]) for i in range(2)]; sq = alloc(st, "sqA", [128, 8, 96])
            rt4 = alloc(st, "rt4", [128, 4, 8, 16])
            qb = alloc(st, "qb", [128, 8, 96], BF16); kbb = alloc(st, "kbb", [128, 8, 96], BF16)
            qTs = alloc(st, "qTs", [128, 8, 128], BF16); kTs = alloc(st, "kTs", [128, 8, 128], BF16)
            vEt = alloc(st, "vEt", [128, 8, 128], BF16)
            pfs2 = [alloc(st, "pfs%d" % i, [128, 512]) for i in range(2)]; his = alloc(st, "his", [128, 512], BF16); sgs = alloc(st, "sgs", [128, 512], BF16)
            ptr = palloc(st, "ptrA", [128, 1024], BF16)
            ptr1 = palloc(st, "ptrA1", [128, 1024], BF16)
            p1 = palloc(st, "p1", [128, 512]); p2 = palloc(st, "p2", [128, 512])
            pf = [palloc(st, "pf%d" % i, [128, 512]) for i in range(2)]
            qa = palloc(st, "qa", [128, 512]); qb_ = palloc(st, "qb_", [128, 512])
            if b == 0:
                for ci, c0 in enumerate(range(0, IN_W, 512)):
                    c1 = min(IN_W, c0 + 512)
                    dma("pool", "w%d" % (ci % 2), lambda e, c0=c0, c1=c1: e.dma_start(out=win[:, :, c0:c1], in_=w_in.rearrange("(c p) n -> p c n", p=128)[:, :, c0:c1]),
                        writes=["win%d" % ci])
            else:
                dma("sp", "w0", lambda e: e.dma_start(out=win[:].rearrange("p a b -> p (a b)"), in_=winb_d), reads=["dram_winb"], writes=["win%d" % i for i in range(6)])
            dma("pool", "w0", lambda e: e.dma_start(out=wq[:, 0, :], in_=w_q_up[0:128, :]), writes=["wq"])
            op("dve", lambda e: e.memset(wq[:, 1, :], 0.0), writes=["wq"])
            for i_ in range(2):
                op("dve", lambda e, i_=i_: e.memset(cnTs[i_][:, 1, :], 0.0), writes=["cnT%d" % i_])
            dma("pool", "w1", lambda e: e.dma_start(out=wq[0:64, 1, :], in_=w_q_up[128:192, :]), writes=["wq"])
            dma("pool", "w0", lambda e: e.dma_start(out=wkvu[:], in_=w_kv_up), writes=["wkvu"])
            op("dve", lambda e: e.memset(vEt[:], 1.0), writes=["vEt"])
            dma("sp", "c0", lambda e: e.dma_start(out=posi[:], in_=pos_d[b].rearrange("(t p) -> t p", p=128)), writes=["posi"])
            dma("sp", "c1", lambda e: e.dma_start(out=invf[:], in_=invf_d.partition_broadcast(128)), writes=["invf"])
            op("dve", lambda e: e.tensor_copy(posf[:], posi[:]), reads=["posi"], writes=["posf"])
            op("pe", lambda e: e.transpose(p1[:, 0:NT], posf[:], identf[0:NT, 0:NT]), reads=["posf", "identf"], writes=["p1"])
            op("dve", lambda e: e.tensor_copy(posT[:], p1[:, 0:NT]), reads=["p1"], writes=["posT"])
            for tt in range(NT):
                op("dve", lambda e, tt=tt: e.tensor_scalar(ang[:, tt, 0, :], invf[:], posT[:, tt:tt + 1], None, ALU.mult), reads=["invf", "posT"], writes=["ang"])
            op("dve", lambda e: e.tensor_scalar(ang[:, :, 1, :], ang[:, :, 0, :], math.pi / 2, None, ALU.add), reads=["ang"], writes=["ang"])
            MAGIC = 12582912.0
            op("dve", lambda e: e.tensor_scalar(kk[:], ang[:], 1.0 / (2 * math.pi), MAGIC, ALU.mult, ALU.add), reads=["ang"], writes=["kk"])
            op("dve", lambda e: e.tensor_scalar(kk[:], kk[:], MAGIC, None, ALU.subtract), reads=["kk"], writes=["kk"])
            C1 = 6.28125
            C2 = 2 * math.pi - C1
            op("dve", lambda e: e.scalar_tensor_tensor(ang[:].rearrange("p a b c -> p (a b c)"), kk[:].rearrange("p a b c -> p (a b c)"), -C1,
                                                        ang[:].rearrange("p a b c -> p (a b c)"), ALU.mult, ALU.add), reads=["kk", "ang"], writes=["ang"])
            op("dve", lambda e: e.scalar_tensor_tensor(ang[:].rearrange("p a b c -> p (a b c)"), kk[:].rearrange("p a b c -> p (a b c)"), -C2,
                                                        ang[:].rearrange("p a b c -> p (a b c)"), ALU.mult, ALU.add), reads=["kk", "ang"], writes=["ang"])
            op("dve", lambda e: e.tensor_scalar(ang[:], ang[:], 3.14159, -3.14159, ALU.min, ALU.max), reads=["ang"], writes=["ang"])
            op("act", lambda e: e.activation(out=cs[:], in_=ang[:], func=AF.Sin), reads=["ang"], writes=["cs"])

            def qk_finish(src, gain, dstb, dstT, dram, j, tag, dname):
                op("act", lambda e: e.activation(out=sq[:].rearrange("p a b -> p (a b)"), in_=src[:].rearrange("p a b -> p (a b)"), func=AF.Square), reads=[tag], writes=["sqA"])
                op("dve", lambda e: e.tensor_reduce(msh[:, 0:8], sq[:], AX.X, ALU.add), reads=["sqA"], writes=["msh"])
                op("dve", lambda e: e.tensor_scalar(msh[:, 0:8], msh[:, 0:8], 1.0 / 96.0, None, ALU.mult), reads=["msh"], writes=["msh"])
                rstd_from_ms(msh, 8, "msh")
                op("dve", lambda e: e.tensor_tensor(src[:], src[:], msh[:, 0:8].unsqueeze(2).to_broadcast([128, 8, 96]), ALU.mult), reads=[tag, "msh"], writes=[tag])
                op("dve", lambda e: e.tensor_tensor(src[:], src[:], gain[:], ALU.mult), reads=[tag, rn(gain)], writes=[tag])
                sinb = cs[:, j, 0, :].unsqueeze(1).to_broadcast([128, 8, 16])
                cosb = cs[:, j, 1, :].unsqueeze(1).to_broadcast([128, 8, 16])
                op("dve", lambda e: e.tensor_tensor(rt4[:, 0], src[:, :, 64:80], cosb, ALU.mult), reads=[tag, "cs"], writes=["rt4"])
                op("dve", lambda e: e.tensor_tensor(rt4[:, 1], src[:, :, 80:96], sinb, ALU.mult), reads=[tag, "cs"], writes=["rt4"])
                op("dve", lambda e: e.tensor_tensor(rt4[:, 2], src[:, :, 80:96], cosb, ALU.mult), reads=[tag, "cs"], writes=["rt4"])
                op("dve", lambda e: e.tensor_tensor(rt4[:, 3], src[:, :, 64:80], sinb, ALU.mult), reads=[tag, "cs"], writes=["rt4"])
                op("act", lambda e: e.activation(out=dstb[:, :, 0:64], in_=src[:, :, 0:64], func=AF.Copy), reads=[tag], writes=[rn(dstb)])
                op("dve", lambda e: e.tensor_tensor(dstb[:, :, 64:80], rt4[:, 0], rt4[:, 1], ALU.subtract), reads=["rt4"], writes=[rn(dstb)])
                op("dve", lambda e: e.tensor_tensor(dstb[:, :, 80:96], rt4[:, 2], rt4[:, 3], ALU.add), reads=["rt4"], writes=[rn(dstb)])
                for h in range(8):
                    op("pe", lambda e, h=h: e.transpose(ptr1[0:96, h * 128:(h + 1) * 128], dstb[:, h, :], identb[:]), reads=[rn(dstb), "identb"], writes=["ptrA1"])
                op("act", lambda e: e.activation(out=dstT[0:96].rearrange("p a b -> p (a b)"), in_=ptr1[0:96, :], func=AF.Copy), reads=["ptrA1"], writes=[rn(dstT)])
                dma("pool", "st_" + dname, lambda e: e.dma_start(out=dram[b, :, :, j * 128:(j + 1) * 128].rearrange("h d t -> d h t"), in_=dstT[0:96]),
                    reads=[rn(dstT)], writes=["dram_" + dname])

            def loadx(j):
                xs = xt[j % 2]
                dma("sp", "ldx%d" % (j % 2), lambda e, xs=xs, j=j: e.dma_start(out=xs[:], in_=x_d[b, j * 128:(j + 1) * 128, :]), writes=[rn(xs)])

            WIN = ["win%d" % i for i in range(6)]

            def stageA0(j):
                par = j % 2
                xs = xt[par]
                xn = rn(xs)
                cnT = cnTs[par]; cnTn = "cnT%d" % par; kf = kfs[par]; kfn = "kf%d" % par
                gp = (j // 4) % 2; jj = j % 4
                hT4 = hT4s[gp]; hTn = "hT4_%d_%d" % (gp, jj)
                hTv = hT4[:, :, jj * 128:(jj + 1) * 128]
                if j == 0:
                    loadx(0)
                if j + 1 < NT:
                    loadx(j + 1)
                op("act", lambda e: e.activation(out=junk[:], in_=xs[:], func=AF.Square, scale=1.0 / 32.0), reads=[xn], writes=["junkA"])
                op("dve", lambda e: e.tensor_reduce(ms[:, 0:1], junk[:], AX.X, ALU.add), reads=["junkA"], writes=["msA"])
                rstd_from_ms(ms, 1, "msA")
                op("dve", lambda e: e.scalar_tensor_tensor(hb[:], xs[:], ms[:, 0:1], g_mix[:], ALU.mult, ALU.mult), reads=[xn, "msA", "g_mix"], writes=["hb"])
                for kc in range(8):
                    op("pe", lambda e, kc=kc: e.transpose(ptr[:, kc * 128:(kc + 1) * 128], hb[:, kc * 128:(kc + 1) * 128], identb[:]), reads=["hb", "identb"], writes=["ptrA"])
                op("act", lambda e: e.activation(out=hTv, in_=ptr[:].rearrange("p (a b) -> p a b", a=8), func=AF.Copy), reads=["ptrA"], writes=[hTn])
                for kc in range(8):
                    op("pe", lambda e, kc=kc: e.matmul(p1[:, 0:352], hTv[:, kc, :], win[:, kc, 0:352], start=(kc == 0), stop=(kc == 7)), reads=[hTn] + WIN, writes=["p1"])
                for kc in range(8):
                    op("pe", lambda e, kc=kc: e.matmul(p2[:], hTv[:, kc, :], win[:, kc, 1888:2400], start=(kc == 0), stop=(kc == 7)), reads=[hTn] + WIN, writes=["p2"])
                op("act", lambda e: e.activation(out=his[:], in_=p2[:], func=AF.Copy), reads=["p2"], writes=["his"])
                dma("pool", "st_hi", lambda e: e.dma_start(out=hi_d[b, j * 128:(j + 1) * 128, :], in_=his[:]), reads=["his"], writes=["dram_hi"])
                for kc in range(8):
                    op("pe", lambda e, kc=kc: e.matmul(p2[:], hTv[:, kc, :], win[:, kc, 2400:2912], start=(kc == 0), stop=(kc == 7)), reads=[hTn] + WIN, writes=["p2"])
                op("act", lambda e: e.activation(out=sgs[:], in_=p2[:], func=AF.Copy), reads=["p2"], writes=["sgs"])
                dma("pool", "st_sg", lambda e: e.dma_start(out=sg_d[b, j * 128:(j + 1) * 128, :], in_=sgs[:]), reads=["sgs"], writes=["dram_sg"])
                op("act", lambda e: e.activation(out=junk[:, 0:192], in_=p1[:, 0:192], func=AF.Square, scale=192.0 ** -0.5), reads=["p1"], writes=["junkA"])
                op("dve", lambda e: e.tensor_reduce(ms[:, 1:2], junk[:, 0:192], AX.X, ALU.add), reads=["junkA"], writes=["msA"])
                op("act", lambda e: e.activation(out=junk[:, 0:128], in_=p1[:, 192:320], func=AF.Square, scale=128.0 ** -0.5), reads=["p1"], writes=["junkA"])
                op("dve", lambda e: e.tensor_reduce(ms[:, 2:3], junk[:, 0:128], AX.X, ALU.add), reads=["junkA"], writes=["msA"])
                op("act", lambda e: e.activation(out=ms[:, 1:3], in_=ms[:, 1:3], func=AF.Ln, bias=epsb[:, 0:1]), reads=["msA", "epsb"], writes=["msA"])
                op("act", lambda e: e.activation(out=ms[:, 1:3], in_=ms[:, 1:3], func=AF.Exp, scale=-0.5), reads=["msA"], writes=["msA"])
                op("dve", lambda e: e.scalar_tensor_tensor(cn[:, 0:192], p1[:, 0:192], ms[:, 1:2], g_qa[:], ALU.mult, ALU.mult), reads=["p1", "msA", "g_qa"], writes=["cn"])
                op("dve", lambda e: e.scalar_tensor_tensor(cn[:, 192:320], p1[:, 192:320], ms[:, 2:3], g_kva[:], ALU.mult, ALU.mult), reads=["p1", "msA", "g_kva"], writes=["cn"])
                op("dve", lambda e: e.tensor_copy(krs[:], p1[:, 320:352]), reads=["p1"], writes=["krs"])
                op("dve", lambda e: e.tensor_copy(kf[:, :, 64:96], krs[:].unsqueeze(1).to_broadcast([128, 8, 32])), reads=["krs"], writes=[kfn])
                op("pe", lambda e: e.transpose(ptr[:, 0:128], cn[:, 0:128], identb[:]), reads=["cn", "identb"], writes=["ptrA"])
                op("pe", lambda e: e.transpose(ptr[0:64, 128:256], cn[:, 128:192], identb[:]), reads=["cn", "identb"], writes=["ptrA"])
                op("pe", lambda e: e.transpose(ptr[:, 256:384], cn[:, 192:320], identb[:]), reads=["cn", "identb"], writes=["ptrA"])
                op("act", lambda e: e.activation(out=cnT[:, 0, :], in_=ptr[:, 0:128], func=AF.Copy), reads=["ptrA"], writes=[cnTn])
                op("act", lambda e: e.activation(out=cnT[0:64, 1, :], in_=ptr[0:64, 128:256], func=AF.Copy), reads=["ptrA"], writes=[cnTn])
                op("act", lambda e: e.activation(out=cnT[:, 2, :], in_=ptr[:, 256:384], func=AF.Copy), reads=["ptrA"], writes=[cnTn])

            def stageF(G, quarter):
                gp = G % 2
                hT4 = hT4s[gp]
                hTns = ["hT4_%d_%d" % (gp, q_) for q_ in range(4)]
                ntok = min(4, NT - 4 * G) * 128
                for c in range(3 * quarter, 3 * quarter + 3):
                    pfb = pf[c % 2]; pfs_ = pfs2[c % 2]
                    for kc in range(8):
                        op("pe", lambda e, c=c, kc=kc, pfb=pfb: e.matmul(pfb[:, 0:ntok], win[:, kc, 352 + c * 128:352 + (c + 1) * 128], hT4[:, kc, 0:ntok], start=(kc == 0), stop=(kc == 7)),
                           reads=hTns + WIN, writes=[rn(pfb)])
                    op("dve", lambda e, pfb=pfb, pfs_=pfs_: e.tensor_copy(pfs_[:, 0:ntok], pfb[:, 0:ntok]), reads=[rn(pfb)], writes=[rn(pfs_)])
                    dma("pool", "st_pf%d" % (c % 2), lambda e, c=c, pfs_=pfs_: e.dma_start(out=pT_d[b, c, :, G * 512:G * 512 + ntok], in_=pfs_[:, 0:ntok]), reads=[rn(pfs_)], writes=["dram_pT"])

            def stageA1(j):
                par = j % 2
                cnT = cnTs[par]; cnTn = "cnT%d" % par; kf = kfs[par]; kfn = "kf%d" % par
                for g, pt in enumerate((qa, qb_)):
                    op("pe", lambda e, g=g, pt=pt: e.matmul(pt[:, 0:384], cnT[:, 0, :], wq[:, 0, g * 384:(g + 1) * 384], start=True, stop=False), reads=[cnTn, "wq"], writes=[rn(pt)])
                    op("pe", lambda e, g=g, pt=pt: e.matmul(pt[:, 0:384], cnT[:, 1, :], wq[:, 1, g * 384:(g + 1) * 384], start=False, stop=True), reads=[cnTn, "wq"], writes=[rn(pt)])
                    op("act", lambda e, g=g, pt=pt: e.activation(out=qf[:, 4 * g:4 * g + 4, :].rearrange("p a b -> p (a b)"), in_=pt[:, 0:384], func=AF.Copy), reads=[rn(pt)], writes=["qf"])
                for g, pt in enumerate((qa, qb_)):
                    op("pe", lambda e, g=g, pt=pt: e.matmul(pt[:], cnT[:, 2, :], wkvu[:, g * 512:(g + 1) * 512], start=True, stop=True), reads=[cnTn, "wkvu"], writes=[rn(pt)])
                    op("act", lambda e, g=g, pt=pt: e.activation(out=kf[:, 4 * g:4 * g + 4, 0:64], in_=pt[:].rearrange("p (a b) -> p a b", a=4)[:, :, 0:64], func=AF.Copy),
                       reads=[rn(pt)], writes=[kfn])
                    op("act", lambda e, g=g, pt=pt: e.activation(out=vEt[:, 4 * g:4 * g + 4, 0:64], in_=pt[:].rearrange("p (a b) -> p a b", a=4)[:, :, 64:128], func=AF.Copy),
                       reads=[rn(pt)], writes=["vEt"])
                dma("pool", "st_vE", lambda e: e.dma_start(out=vE_d[b, :, j * 128:(j + 1) * 128, :].rearrange("h t c -> t h c"), in_=vEt[:]), reads=["vEt"], writes=["dram_vE"])
                qk_finish(qf, gq96, qb, qTs, qT_d, j, "qf", "qf")
                qk_finish(kf, gk96, kbb, kTs, kT_d, j, kfn, "kf")

            NG = (NT + 3) // 4
            fq = []
            for t in range(NT + 1):
                if t >= 1 and (t % 4 == 0 or t == NT):
                    Gd = (t - 1) // 4
                    fq.extend((Gd, q_) for q_ in range(4))
                A_ = record(stageA1, t - 1) if t >= 1 else []
                B_ = record(stageA0, t) if t < NT else []
                C_ = record(stageF, *fq.pop(0)) if fq else []
                emit_zip(A_, B_, C_)
            while fq:
                emit_zip(record(stageF, *fq.pop(0)))
            Sx.flush()

        if dbg == "A":
            continue
        with contextlib.ExitStack() as st:
            kTh = [alloc(st, "kTh%d" % i, [128, S], BF16) for i in range(2)]
            qTh = [alloc(st, "qTh%d" % i, [128, S], BF16) for i in range(2)]
            vEh = [alloc(st, "vEh%d" % i, [128, NT, 128], BF16) for i in range(2)]
            pTt = [alloc(st, "pTt%d" % i, [128, 2 * QC], BF16) for i in range(3)]
            rc = alloc(st, "rc", [128, QC])
            atmp = [alloc(st, "atmp%d" % i, [128, QC], BF16) for i in range(2)]
            sc = [palloc(st, "sc%d" % i, [128, 1024]) for i in range(3)]
            oa = [palloc(st, "oa%d" % i, [128, 512]) for i in range(2)]
            cgu = [alloc(st, "cgu%d" % i, [128, 8, 512], BF16) for i in range(2)]
            cdn = [alloc(st, "cdn%d" % i, [128, 2, D], BF16) for i in range(2)]
            for ex in range(b * (NEXP // NB), (b + 1) * (NEXP // NB)):
                cs_ = ex % 2
                dma("pool", "cv0", lambda e, ex=ex, cs_=cs_: e.dma_start(out=cgu[cs_][:, :, 0:256], in_=w_gate[ex].rearrange("(c p) h -> p c h", p=128)), writes=["cgu_g%d" % cs_])
                dma("pool", "cv1", lambda e, ex=ex, cs_=cs_: e.dma_start(out=cgu[cs_][:, :, 256:512], in_=w_up[ex].rearrange("(c p) h -> p c h", p=128)), writes=["cgu_u%d" % cs_])
                dma("pool", "cv2", lambda e, ex=ex, cs_=cs_: e.dma_start(out=cdn[cs_][:], in_=w_down[ex].rearrange("(c p) d -> p c d", p=128)), writes=["cdn%d" % cs_])
                dma("pool", "cv3", lambda e, ex=ex, cs_=cs_: e.dma_start(out=wgub_d[ex], in_=cgu[cs_][:].rearrange("p a b -> p (a b)")),
                    reads=["cgu_g%d" % cs_, "cgu_u%d" % cs_], writes=["dram_wgub"])
                dma("pool", "cv4", lambda e, ex=ex, cs_=cs_: e.dma_start(out=wdb_d[ex], in_=cdn[cs_][:].rearrange("p a b -> p (a b)")), reads=["cdn%d" % cs_], writes=["dram_wdb"])
            if b == 0:
                stg = alloc(st, "stgW", [128, 8, D], BF16)
                for wi, wsrc in enumerate((w_out, x_w_q, x_w_o)):
                    dma("pool", "cv5", lambda e, wsrc=wsrc: e.dma_start(out=stg[:], in_=wsrc.rearrange("(c p) n -> p c n", p=128)), writes=["stgW"])
                    dma("pool", "cv6", lambda e, wi=wi: e.dma_start(out=wcb_d[wi], in_=stg[:].rearrange("p a b -> p (a b)")), reads=["stgW"], writes=["dram_wcb"])
                for c0 in range(0, IN_W, 512):
                    c1 = min(IN_W, c0 + 512)
                    dma("pool", "cv5", lambda e, c0=c0, c1=c1: e.dma_start(out=stg[:, :, 0:c1 - c0], in_=w_in.rearrange("(c p) n -> p c n", p=128)[:, :, c0:c1]), writes=["stgW"])
                    dma("pool", "cv6", lambda e, c0=c0, c1=c1: e.dma_start(out=winb_d.rearrange("p (c n) -> p c n", c=8)[:, :, c0:c1], in_=stg[:, :, 0:c1 - c0]), reads=["stgW"], writes=["dram_winb"])
            NP = NT // 2
            steps = [(h, qc, kp) for h in range(8) for qc in range(NQC) for kp in range(NP)]
            nst = len(steps)
            LOOK = 2

            def mla_loads(h):
                sl = h % 2
                dma("sp", "lk%d" % sl, lambda e: e.dma_start(out=kTh[sl][0:96, :], in_=kT_d[b, h]), reads=["dram_kf"], writes=["kTh%d" % sl])
                dma("sp", "lq%d" % sl, lambda e: e.dma_start(out=qTh[sl][0:96, :], in_=qT_d[b, h]), reads=["dram_qf"], writes=["qTh%d" % sl])
                dma("sp", "lv%d" % sl, lambda e: e.dma_start(out=vEh[sl][:], in_=vE_d[b, h].rearrange("(n p) c -> p n c", p=128)), reads=["dram_vE"], writes=["vEh%d" % sl])

            def mla_score(i):
                h, qc, kp = steps[i]
                sl = h % 2
                s_ = sc[i % 3]
                for u in range(2):
                    kt = 2 * kp + u
                    op("pe", lambda e, kt=kt, u=u: e.matmul(s_[:, u * 512:u * 512 + QC], kTh[sl][0:96, kt * 128:(kt + 1) * 128], qTh[sl][0:96, qc * QC:(qc + 1) * QC], start=True, stop=True),
                       reads=["kTh%d" % sl, "qTh%d" % sl], writes=[rn(s_)])

            def mla_exp_pv(i):
                h, qc, kp = steps[i]
                sl = h % 2
                s_ = sc[i % 3]
                p_ = pTt[i % 3]
                o_ = oa[(h * NQC + qc) % 2]
                if QC == 512:
                    op("act", lambda e: e.activation(out=p_[:], in_=s_[:], func=AF.Exp), reads=[rn(s_)], writes=[rn(p_)])
                else:
                    op("act", lambda e: e.activation(out=p_[:].rearrange("p (u q) -> p u q", u=2), in_=s_[:].rearrange("p (u q) -> p u q", u=2)[:, :, 0:QC], func=AF.Exp),
                       reads=[rn(s_)], writes=[rn(p_)])
                for u in range(2):
                    kt = 2 * kp + u
                    op("pe", lambda e, kt=kt, u=u: e.matmul(o_[:, 0:QC], vEh[sl][:, kt, :], p_[:, u * QC:(u + 1) * QC], start=(kt == 0), stop=(kt == NT - 1)),
                       reads=[rn(p_), "vEh%d" % sl], writes=[rn(o_)])
                if kp == NP - 1:
                    op("dve", lambda e: e.reciprocal(rc[0:64, :], o_[64:128, 0:QC]), reads=[rn(o_)], writes=["rc"])
                    tm_ = atmp[(h * NQC + qc) % 2]
                    op("dve", lambda e: e.tensor_tensor(tm_[0:64, :], o_[0:64, 0:QC], rc[0:64, :], ALU.mult), reads=[rn(o_), "rc"], writes=[rn(tm_)])
                    dma("sp", "ash%d" % ((h * NQC + qc) % 2), lambda e: e.dma_start(out=aT_d[b, h // 2, (h % 2) * 64:(h % 2) * 64 + 64, qc * QC:(qc + 1) * QC], in_=tm_[0:64, :]),
                        reads=[rn(tm_)], writes=["dram_aT"])

            mla_loads(0)
            mla_loads(1)
            for i in range(min(LOOK, nst)):
                mla_score(i)
            for i in range(nst):
                h, qc, kp = steps[i]
                if qc == 0 and kp == 0 and 1 <= h and h + 1 < 8:
                    mla_loads(h + 1)
                if i + LOOK < nst:
                    mla_score(i + LOOK)
                mla_exp_pv(i)
            Sx.flush()

        oacc_stack = contextlib.ExitStack()
        oacc = alloc(oacc_stack, "oacc", [128, NT, 512], BF16)
        with contextlib.ExitStack() as st:
            vhs = [alloc(st, "vh%d" % i, [128, NT, 128], BF16) for i in range(2)]
            qc_ = [alloc(st, "qchk%d" % d, [128, S], BF16) for d in range(4)]
            kc_ = [alloc(st, "kchk%d" % d, [128, S], BF16) for d in range(4)]
            eC = [alloc(st, "eC%d" % d, [128, NT]) for d in range(4)]
            zq = alloc(st, "zq", [128, BLK]); thq = alloc(st, "thq", [128, BLK]); sil = alloc(st, "sil", [128, BLK]); t1 = alloc(st, "t1h", [128, BLK])
            zfs = [alloc(st, "zf%d" % d, [128, BLK]) for d in range(2)]; lgfs = [alloc(st, "lgf%d" % d, [128, BLK]) for d in range(2)]
            bbs = [alloc(st, "bb%d" % d, [128, BLK]) for d in range(2)]; wws = [alloc(st, "ww%d" % d, [128, BLK]) for d in range(2)]
            rws = [alloc(st, "rw%d" % d, [128, BLK]) for d in range(2)]
            rmask = alloc(st, "rmask", [128, BLK])
            Sst = [alloc(st, "Sst%d" % d, [128, 128]) for d in range(4)]
            Sdf = [alloc(st, "Sdf%d" % d, [128, 128]) for d in range(4)]
            Sdb = [alloc(st, "Sdb%d" % d, [128, 128], BF16) for d in range(4)]
            ktok = [alloc(st, "ktok%d" % d, [128, 128], BF16) for d in range(4)]
            atm = [alloc(st, "atm%d" % d, [128, 128], BF16) for d in range(4)]
            ptr = palloc(st, "ptrH", [128, 1024], BF16)
            pb = [palloc(st, "pbH%d" % d, [128, 512]) for d in range(4)]
            op("pool", lambda e: e.memset(oacc[:], 0.0), writes=["oacc"])
            SGN = min(NT, 8)
            sgbuf = [alloc(st, "sgbuf%d" % i, [128, SGN, 512], BF16) for i in range(2)]
            for pi in range(NT // SGN):
                sb_ = sgbuf[pi % 2]
                view = sg_d[b, pi * SGN * 128:(pi + 1) * SGN * 128, :].rearrange("(n p) c -> p n c", p=128)
                dma("sp", "sgl%d" % (pi % 2), lambda e, sb_=sb_, view=view: e.dma_start(out=sb_[:], in_=view), reads=["dram_sg"], writes=[rn(sb_)])
                op("act", lambda e, sb_=sb_: e.activation(out=sb_[:], in_=sb_[:], func=AF.Silu), reads=[rn(sb_)], writes=[rn(sb_)])
                dma("sp", "sgs%d" % (pi % 2), lambda e, sb_=sb_, view=view: e.dma_start(out=view, in_=sb_[:]), reads=[rn(sb_)], writes=["dram_sg"])
            op("dve", lambda e: e.memset(rmask[:], 1.0), writes=["rmask"])
            op("dve", lambda e: e.memset(rmask[:].rearrange("p (c t) -> p c t", t=128)[:, :, 0:1], 0.0), writes=["rmask"])
            nbc = BLK // 128
            for hp in range(2):
                for hx in range(2):
                    hh = 2 * hp + hx
                    vh = vhs[hx]
                    dma("sp", "lvh%d" % hx, lambda e, hh=hh, vh=vh: e.dma_start(out=vh[:], in_=hi_d[b, :, hh * 128:(hh + 1) * 128].rearrange("(n p) c -> p n c", p=128)),
                        reads=["dram_hi"], writes=["vh%d" % hx])
                    for blk in range(S // BLK):
                        t0 = blk * BLK
                        dma("sp", "lzq", lambda e, hh=hh, t0=t0: e.dma_start(out=zq[:], in_=pT_d[b, hh, :, t0:t0 + BLK]), reads=["dram_pT"], writes=["zq"])
                        for d in range(2):
                            dma("sp", "lzf%d" % d, lambda e, hh=hh, t0=t0, d=d: e.dma_start(out=zfs[d][:], in_=pT_d[b, 4 + 4 * d + hh, :, t0:t0 + BLK]), reads=["dram_pT"], writes=["zf%d" % d])
                        op("act", lambda e: e.activation(out=thq[:], in_=zq[:], func=AF.Tanh, scale=0.5), reads=["zq"], writes=["thq"])
                        for d in range(2):
                            op("act", lambda e, d=d: e.activation(out=zfs[d][:], in_=zfs[d][:], func=AF.Tanh, scale=0.5), reads=["zf%d" % d], writes=["zf%d" % d])
                        for d in range(2):
                            col = d * 4 + hh
                            op("act", lambda e, d=d, col=col: e.activation(out=lgfs[d][:], in_=zfs[d][:], func=AF.Ln, scale=hoT[:, col:col + 1], bias=lbhT[:, col:col + 1]),
                               reads=["zf%d" % d, "hoT", "lbhT"], writes=["lgf%d" % d])
                        op("dve", lambda e: e.scalar_tensor_tensor(sil[:], thq[:], 1.0, zq[:], ALU.add, ALU.mult), reads=["thq", "zq"], writes=["sil"])
                        for d in range(2):
                            col = d * 4 + hh
                            ch = hx * 2 + d
                            bb = bbs[d]; ww = wws[d]; rw = rws[d]; lgf = lgfs[d]
                            op("dve", lambda e, bb=bb, lgf=lgf: e.tensor_tensor_scan(bb[:], rmask[:], lgf[:], 0.0, ALU.mult, ALU.add), reads=["rmask", "lgf%d" % d], writes=["bb%d" % d])
                            bC = bb[:].rearrange("p (c t) -> p c t", t=128)[:, :, 127:128]
                            op("act", lambda e, ch=ch, blk=blk, bC=bC: e.activation(out=eC[ch][:, blk * nbc:(blk + 1) * nbc].unsqueeze(2), in_=bC, func=AF.Exp),
                               reads=["bb%d" % d], writes=["eC%d" % ch])
                            if d == 0:
                                op("dve", lambda e, bC=bC, bb=bb, ww=ww: e.tensor_tensor(ww[:].rearrange("p (c t) -> p c t", t=128), bC.to_broadcast([128, nbc, 128]),
                                                                                      bb[:].rearrange("p (c t) -> p c t", t=128), ALU.subtract), reads=["bb%d" % d], writes=["ww%d" % d])
                            else:
                                op("dve", lambda e, bb=bb, ww=ww, lgf=lgf: e.tensor_tensor(ww[:], bb[:], lgf[:], ALU.subtract), reads=["bb%d" % d, "lgf%d" % d], writes=["ww%d" % d])
                            op("act", lambda e, ww=ww, rw=rw: e.activation(out=rw[:], in_=ww[:], func=AF.Exp, scale=-1.0), reads=["ww%d" % d], writes=["rw%d" % d])
                            op("act", lambda e, ww=ww: e.activation(out=ww[:], in_=ww[:], func=AF.Exp), reads=["ww%d" % d], writes=["ww%d" % d])
                            op("dve", lambda e, d=d: e.tensor_scalar(t1[:], zfs[d][:], -1.0, 1.0, ALU.mult, ALU.add), reads=["zf%d" % d], writes=["t1h"])
                            op("dve", lambda e, ch=ch, col=col, t0=t0, ww=ww: e.scalar_tensor_tensor(kc_[ch][:, t0:t0 + BLK], t1[:], hoT[:, col:col + 1], ww[:], ALU.mult, ALU.mult),
                               reads=["t1h", "ww%d" % d, "hoT"], writes=["kchk%d" % ch])
                            op("dve", lambda e, ch=ch, t0=t0, rw=rw: e.scalar_tensor_tensor(qc_[ch][:, t0:t0 + BLK], sil[:], 0.5, rw[:], ALU.mult, ALU.mult),
                               reads=["sil", "rw%d" % d], writes=["qchk%d" % ch])
                for ch in range(4):
                    op("dve", lambda e, ch=ch: e.memset(Sst[ch][:], 0.0), writes=["Sst%d" % ch])
                for ci in range(NT):
                    for ch in range(4):
                        hx, d = ch // 2, ch % 2
                        hh = 2 * hp + hx
                        vh = vhs[hx]
                        vhn = "vh%d" % hx
                        c = ci if d == 0 else NT - 1 - ci
                        cs_ = slice(c * 128, (c + 1) * 128)
                        msk = maskf if d == 0 else maskb
                        nm = lambda s, ch=ch: s + str(ch)
                        pat_ = pb[ch][:, 0:128]; po_ = pb[ch][:, 128:256]; pds_ = pb[ch][:, 256:384]
                        op("pe", lambda e, ch=ch, cs_=cs_: e.transpose(ptr[:, ch * 128:(ch + 1) * 128], kc_[ch][:, cs_], identb[:]), reads=[nm("kchk"), "identb"], writes=[nm("ptrH")])
                        op("act", lambda e, ch=ch: e.activation(out=ktok[ch][:], in_=ptr[:, ch * 128:(ch + 1) * 128], func=AF.Copy), reads=[nm("ptrH")], writes=[nm("ktok")])
                        op("pe", lambda e, ch=ch, cs_=cs_, pat_=pat_: e.matmul(pat_, kc_[ch][:, cs_], qc_[ch][:, cs_], start=True, stop=True), reads=[nm("kchk"), nm("qchk")], writes=[nm("pat")])
                        op("dve", lambda e, ch=ch, msk=msk, pat_=pat_: e.tensor_tensor(atm[ch][:], pat_, msk[:], ALU.mult), reads=[nm("pat"), rn(msk)], writes=[nm("atm")])
                        op("dve", lambda e, ch=ch, c=c: e.tensor_scalar(Sdf[ch][:], Sst[ch][:], eC[ch][:, c:c + 1], None, ALU.mult), reads=[nm("Sst"), nm("eC")], writes=[nm("Sdf")])
                        op("act", lambda e, ch=ch, c=c: e.activation(out=Sdb[ch][:], in_=Sst[ch][:], func=AF.Copy, scale=eC[ch][:, c:c + 1]), reads=[nm("Sst"), nm("eC")], writes=[nm("Sdb")])
                        op("pe", lambda e, ch=ch, c=c, vh=vh, po_=po_: e.matmul(po_, atm[ch][:], vh[:, c, :], start=True, stop=False), reads=[nm("atm"), vhn], writes=[nm("poH")])
                        op("pe", lambda e, ch=ch, cs_=cs_, po_=po_: e.matmul(po_, qc_[ch][:, cs_], Sdb[ch][:], start=False, stop=True), reads=[nm("qchk"), nm("Sdb")], writes=[nm("poH")])
                        op("pe", lambda e, ch=ch, c=c, vh=vh, pds_=pds_: e.matmul(pds_, ktok[ch][:], vh[:, c, :], start=True, stop=True), reads=[nm("ktok"), vhn], writes=[nm("pds")])
                        op("dve", lambda e, ch=ch, pds_=pds_: e.tensor_tensor(Sst[ch][:], Sdf[ch][:], pds_, ALU.add), reads=[nm("Sdf"), nm("pds")], writes=[nm("Sst")])
                        oa_ = oacc[:, c, hh * 128:(hh + 1) * 128]
                        oan = "oacc_%d_%d" % (c, hh)
                        op("dve", lambda e, oa_=oa_, po_=po_: e.tensor_tensor(oa_, oa_, po_, ALU.add), reads=[nm("poH"), "oacc", oan], writes=[oan])
            Sx.flush()

        with contextlib.ExitStack() as st:
            wo = alloc(st, "wo", [128, 8, D], BF16); wxq = alloc(st, "wxq", [128, 8, D], BF16); wxo = alloc(st, "wxo", [128, 8, D], BF16)
            xt = [alloc(st, "xtC%d" % i, [128, D]) for i in range(2)]
            sgts = [alloc(st, "sgt%d" % i, [128, 512], BF16) for i in range(2)]; atts = [alloc(st, "att%d" % i, [128, 4, 128], BF16) for i in range(2)]
            junk = alloc(st, "junkC", [128, D], BF16); ms = alloc(st, "msC", [128, 8]); junk1 = alloc(st, "junkC1", [128, D]); ms1 = alloc(st, "msC1", [128, 8])
            of = alloc(st, "of", [128, 4, 128]); rb = alloc(st, "rb", [128, 512], BF16); rT = alloc(st, "rT", [128, 4, 128], BF16)
            x1s = [alloc(st, "x1_%d" % i, [128, D]) for i in range(2)]; h2 = alloc(st, "h2", [128, D], BF16); h2T = alloc(st, "h2T", [128, 8, 128], BF16)
            qx = alloc(st, "qx", [128, 4, 256]); qxb = alloc(st, "qxb", [128, 4, 256], BF16); qxTs = [alloc(st, "qxT_%d" % i, [128, 8, 128], BF16) for i in range(2)]
            pTx = alloc(st, "pTx", [128, 8, 128], BF16); rs = alloc(st, "rs", [128, 4, 128]); oxT = alloc(st, "oxT", [128, 8, 128], BF16)
            x2s = [alloc(st, "x2t_%d" % i, [128, D]) for i in range(2)]; h3f = alloc(st, "h3f", [128, D]); h3bs = [alloc(st, "h3b_%d" % i, [128, D], BF16) for i in range(2)]; h3T = alloc(st, "h3T", [128, 8, 128])
            lg = alloc(st, "lg", [128, 72]); sm = alloc(st, "smC", [128, 16]); gm = alloc(st, "gm", [128, 8]); elm = alloc(st, "elm", [128, 8, 8])
            top8 = alloc(st, "top8", [128, 8]); A1 = alloc(st, "A1", [128, 64]); A2 = alloc(st, "A2", [128, 64]); Ab = alloc(st, "Ab", [128, 64], BF16)
            posn = alloc(st, "posn", [128, 64]); jk64 = alloc(st, "jk64", [128, 64])
            ptr = palloc(st, "ptrC", [128, 1024], BF16)
            py = palloc(st, "py", [128, 1024])
            pss = palloc(st, "pss", [128, 1024])
            psm = palloc(st, "psm", [128, 512])
            pr = palloc(st, "prC", [128, 512])
            pl = palloc(st, "plC", [128, 512])
            dma("sp", "w0", lambda e: e.dma_start(out=wo[:].rearrange("p a b -> p (a b)"), in_=wcb_d[0]), reads=["dram_wcb"], writes=["wo"])
            dma("sp", "w1", lambda e: e.dma_start(out=wxq[:].rearrange("p a b -> p (a b)"), in_=wcb_d[1]), reads=["dram_wcb"], writes=["wxq"])
            dma("sp", "w2", lambda e: e.dma_start(out=wxo[:].rearrange("p a b -> p (a b)"), in_=wcb_d[2]), reads=["dram_wcb"], writes=["wxo"])

            def transpose8(src, dst, dtag, stag):
                for kc in range(8):
                    op("pe", lambda e, kc=kc: e.transpose(ptr[:, kc * 128:(kc + 1) * 128], src[:, kc * 128:(kc + 1) * 128], identb[:]), reads=[stag, "identb"], writes=["ptrC"])
                op("act", lambda e: e.activation(out=dst[:].rearrange("p a b -> p (a b)"), in_=ptr[:], func=AF.Copy), reads=["ptrC"], writes=[dtag])

            def rms_full(src, stag, gain, dst, dtag):
                op("act", lambda e: e.activation(out=junk[:], in_=src[:], func=AF.Square, scale=1.0 / 32.0), reads=[stag], writes=["junkC"])
                op("dve", lambda e: e.tensor_reduce(ms[:, 0:1], junk[:], AX.X, ALU.add), reads=["junkC"], writes=["msC"])
                rstd_from_ms(ms, 1, "msC")
                op("dve", lambda e: e.scalar_tensor_tensor(dst[:], src[:], ms[:, 0:1], gain[:], ALU.mult, ALU.mult), reads=[stag, "msC", rn(gain)], writes=[dtag])

            def cload(j):
                p_ = j % 2
                xs_ = xt[p_]
                dma("sp", "ldx%d" % p_, lambda e: e.dma_start(out=xs_[:], in_=x_d[b, j * 128:(j + 1) * 128, :]), writes=[rn(xs_)])
                dma("sp", "ldat%d" % p_, lambda e: e.dma_start(out=atts[p_][:], in_=aT_d[b, :, :, j * 128:(j + 1) * 128].rearrange("c p t -> p c t")), reads=["dram_aT"], writes=["att%d" % p_])
                dma("sp", "ldsg%d" % p_, lambda e: e.dma_start(out=sgts[p_][:], in_=sg_d[b, j * 128:(j + 1) * 128, :]), reads=["dram_sg"], writes=["sgt%d" % p_])

            def stage0(j):
                g = b * NT + j
                xs = xt[j % 2]
                xn = rn(xs)
                par = j % 2
                x1 = x1s[par]; x1n = "x1_%d" % par; qxT = qxTs[par]; qxTn = "qxT_%d" % par
                att = atts[par]; sgt = sgts[par]; attn = "att%d" % par; sgtn = "sgt%d" % par
                if j == 0:
                    cload(0)
                if j + 1 < NT:
                    cload(j + 1)
                for hh in range(4):
                    op("act", lambda e, hh=hh, j=j: e.activation(out=junk[:, 0:128], in_=oacc[:, j, hh * 128:(hh + 1) * 128], func=AF.Square, scale=128.0 ** -0.5), reads=["oacc"] + ["oacc_%d_%d" % (j, q_) for q_ in range(4)], writes=["junkC"])
                    op("dve", lambda e, hh=hh, j=j: e.tensor_reduce(ms[:, 4 + hh:5 + hh], junk[:, 0:128], AX.X, ALU.add), reads=["junkC"], writes=["msC"])
                op("act", lambda e: e.activation(out=ms[:, 4:8], in_=ms[:, 4:8], func=AF.Ln, bias=epsb[:, 0:1]), reads=["msC", "epsb"], writes=["msC"])
                op("act", lambda e: e.activation(out=ms[:, 4:8], in_=ms[:, 4:8], func=AF.Exp, scale=-0.5), reads=["msC"], writes=["msC"])
                op("dve", lambda e, j=j: e.tensor_tensor(of[:], oacc[:, j, :].rearrange("p (a b) -> p a b", a=4), ms[:, 4:8].unsqueeze(2).to_broadcast([128, 4, 128]), ALU.mult),
                   reads=["oacc", "msC"] + ["oacc_%d_%d" % (j, q_) for q_ in range(4)], writes=["of"])
                op("dve", lambda e: e.tensor_tensor(of[:], of[:], g_o[:], ALU.mult), reads=["of", "g_o"], writes=["of"])
                op("dve", lambda e: e.tensor_tensor(rb[:], of[:].rearrange("p a b -> p (a b)"), sgt[:], ALU.mult), reads=["of", sgtn], writes=["rb"])
                for c in range(4):
                    op("pe", lambda e, c=c: e.transpose(ptr[:, c * 128:(c + 1) * 128], rb[:, c * 128:(c + 1) * 128], identb[:]), reads=["rb", "identb"], writes=["ptrC"])
                op("act", lambda e: e.activation(out=rT[:].rearrange("p a b -> p (a b)"), in_=ptr[:, 0:512], func=AF.Copy), reads=["ptrC"], writes=["rT"])
                for hf in range(2):
                    for c in range(8):
                        lhs = att[:, c, :] if c < 4 else rT[:, c - 4, :]
                        op("pe", lambda e, hf=hf, c=c, lhs=lhs: e.matmul(py[:, hf * 512:(hf + 1) * 512], lhs, wo[:, c, hf * 512:(hf + 1) * 512], start=(c == 0), stop=(c == 7)),
                           reads=[attn, "rT", "wo"], writes=["py"])
                op("dve", lambda e, xs=xs: e.tensor_tensor(x1[:], py[:], xs[:], ALU.add), reads=["py", xn], writes=[x1n])
                if dbg:
                    dma("sp", "dbg1", lambda e, g=g: e.dma_start(out=dx1_d[g * 128:(g + 1) * 128, :], in_=x1[:]), reads=[x1n])
                    dma("sp", "dbg2", lambda e, g=g: e.dma_start(out=dr_d[g * 128:(g + 1) * 128, :], in_=rb[:]), reads=["rb"])
                rms_full(x1, x1n, g_cross, h2, "h2")
                transpose8(h2, h2T, "h2T", "h2")
                for hf in range(2):
                    for c in range(8):
                        op("pe", lambda e, hf=hf, c=c: e.matmul(py[:, hf * 512:(hf + 1) * 512], h2T[:, c, :], wxq[:, c, hf * 512:(hf + 1) * 512], start=(c == 0), stop=(c == 7)),
                           reads=["h2T", "wxq"], writes=["py"])
                op("act", lambda e: e.activation(out=qx[:].rearrange("p a b -> p (a b)"), in_=py[:], func=AF.Copy), reads=["py"], writes=["qx"])
                for hh in range(4):
                    op("act", lambda e, hh=hh: e.activation(out=junk[:, 0:256], in_=qx[:, hh, :], func=AF.Square, scale=1.0 / 16.0),
                       reads=["qx"], writes=["junkC"])
                    op("dve", lambda e, hh=hh: e.tensor_reduce(ms[:, hh:hh + 1], junk[:, 0:256], AX.X, ALU.add), reads=["junkC"], writes=["msC"])
                rstd_from_ms(ms, 4, "msC")
                op("dve", lambda e: e.tensor_tensor(qx[:], qx[:], ms[:, 0:4].unsqueeze(2).to_broadcast([128, 4, 256]), ALU.mult), reads=["qx", "msC"], writes=["qx"])
                op("dve", lambda e: e.tensor_tensor(qxb[:], qx[:], g_xq[:], ALU.mult), reads=["qx", "g_xq"], writes=["qxb"])
                transpose8(qxb[:].rearrange("p a b -> p (a b)") if False else qxb, qxT, "qxT", "qxb") if False else None
                for kc in range(8):
                    op("pe", lambda e, kc=kc: e.transpose(ptr[:, kc * 128:(kc + 1) * 128], qxb[:, kc // 2, (kc % 2) * 128:(kc % 2 + 1) * 128], identb[:]), reads=["qxb", "identb"], writes=["ptrC"])
                op("act", lambda e: e.activation(out=qxT[:].rearrange("p a b -> p (a b)"), in_=ptr[:], func=AF.Copy), reads=["ptrC"], writes=[qxTn])

            def stage1(j):
                g = b * NT + j
                xs = xt[j % 2]
                xn = rn(xs)
                par = j % 2
                x1 = x1s[par]; x1n = "x1_%d" % par; qxT = qxTs[par]; qxTn = "qxT_%d" % par
                x2 = x2s[par]; x2n = "x2t_%d" % par; h3b = h3bs[par]; h3bn = "h3b_%d" % par
                for hh in range(4):
                    for kt in range(2):
                        for dc in range(2):
                            op("pe", lambda e, hh=hh, kt=kt, dc=dc: e.matmul(pss[:, (hh * 2 + kt) * 128:(hh * 2 + kt + 1) * 128], xKT[:, b, hh * 2 + dc, kt * 128:(kt + 1) * 128],
                                                                           qxT[:, hh * 2 + dc, :], start=(dc == 0), stop=(dc == 1)), reads=["xKT", qxTn], writes=["pss"])
                op("act", lambda e: e.activation(out=pTx[:].rearrange("p a b -> p (a b)"), in_=pss[:], func=AF.Exp), reads=["pss"], writes=["pTx"])
                for kt in range(2):
                    op("pe", lambda e, kt=kt: e.matmul(psm[:].rearrange("p (a b) -> p a b", a=4), onesb[:], pTx[:].rearrange("p (h k) t -> p h k t", k=2)[:, :, kt, :],
                                                      start=(kt == 0), stop=(kt == 1)), reads=["pTx", "onesb"], writes=["psm"])
                for hh in range(4):
                    for dc in range(2):
                        for kt in range(2):
                            op("pe", lambda e, hh=hh, kt=kt, dc=dc: e.matmul(pss[:, (hh * 2 + dc) * 128:(hh * 2 + dc + 1) * 128], xV[:, b, kt, hh * 256 + dc * 128:hh * 256 + (dc + 1) * 128],
                                                                           pTx[:, hh * 2 + kt, :], start=(kt == 0), stop=(kt == 1)), reads=["xV", "pTx"], writes=["pss"])
                op("act", lambda e: e.activation(out=rs[:].rearrange("p a b -> p (a b)"), in_=psm[:], func=AF.Ln), reads=["psm"], writes=["rs"])
                op("act", lambda e: e.activation(out=rs[:].rearrange("p a b -> p (a b)"), in_=rs[:].rearrange("p a b -> p (a b)"), func=AF.Exp, scale=-1.0), reads=["rs"], writes=["rs"])
                for dc in range(2):
                    op("dve", lambda e, dc=dc: e.tensor_tensor(oxT[:].rearrange("p (h k) t -> p h k t", k=2)[:, :, dc, :], pss[:].rearrange("p (h k t) -> p h k t", k=2, t=128)[:, :, dc, :],
                                                              rs[:], ALU.mult), reads=["pss", "rs"], writes=["oxT"])
                for hf in range(2):
                    for c in range(8):
                        op("pe", lambda e, hf=hf, c=c: e.matmul(pss[:, hf * 512:(hf + 1) * 512], oxT[:, c, :], wxo[:, c, hf * 512:(hf + 1) * 512], start=(c == 0), stop=(c == 7)),
                           reads=["oxT", "wxo"], writes=["pss"])
                op("dve", lambda e: e.tensor_tensor(x2[:], pss[:], x1[:], ALU.add), reads=["pss", x1n], writes=[x2n])
                dma("pool", "st_x2", lambda e, g=g: e.dma_start(out=x2_d[g * 128:(g + 1) * 128, :], in_=x2[:]), reads=[x2n], writes=["dram_x2"])

            def stage2(j):
                g = b * NT + j
                par = j % 2
                x2 = x2s[par]; x2n = "x2t_%d" % par; h3b = h3bs[par]; h3bn = "h3b_%d" % par
                op("act", lambda e: e.activation(out=junk1[:], in_=x2[:], func=AF.Square, scale=1.0 / 32.0), reads=[x2n], writes=["junkC1"])
                op("dve", lambda e: e.tensor_reduce(ms1[:, 0:1], junk1[:], AX.X, ALU.add), reads=["junkC1"], writes=["msC1"])
                rstd_from_ms(ms1, 1, "msC1")
                op("dve", lambda e: e.scalar_tensor_tensor(h3f[:], x2[:], ms1[:, 0:1], g_ffn[:], ALU.mult, ALU.mult), reads=[x2n, "msC1", "g_ffn"], writes=["h3f"])
                op("act", lambda e: e.activation(out=h3b[:], in_=h3f[:], func=AF.Copy), reads=["h3f"], writes=[h3bn])
                dma("pool", "st_h3", lambda e, g=g: e.dma_start(out=h3_d[g * 128:(g + 1) * 128, :], in_=h3b[:]), reads=[h3bn], writes=["dram_h3"])
                for hf in range(2):
                    for kc in range(4):
                        c = hf * 4 + kc
                        op("pe", lambda e, c=c, kc=kc: e.transpose(pr[:, kc * 128:(kc + 1) * 128], h3f[:, c * 128:(c + 1) * 128], identf[:]), reads=["h3f", "identf"], writes=["prC"])
                    op("act", lambda e, hf=hf: e.activation(out=h3T[:, hf * 4:(hf + 1) * 4, :].rearrange("p a b -> p (a b)"), in_=pr[:], func=AF.Copy), reads=["prC"], writes=["h3T"])
                for c in range(8):
                    op("pe", lambda e, c=c: e.matmul(pl[:, 0:72], h3T[:, c, :], wrt[:, c, :], start=(c == 0), stop=(c == 7)), reads=["h3T", "wrt"], writes=["plC"])
                op("dve", lambda e: e.tensor_tensor(lg[:], pl[:, 0:72], b_r[:], ALU.add), reads=["plC", "b_r"], writes=["lg"])
                op("dve", lambda e: e.tensor_reduce(sm[:, 0:1], lg[:, 0:8], AX.X, ALU.max), reads=["lg"], writes=["smC"])
                op("dve", lambda e: e.tensor_scalar(gm[:], lg[:, 0:8], sm[:, 0:1], None, ALU.is_ge), reads=["lg", "smC"], writes=["gm"])
                op("dve", lambda e: e.tensor_scalar(sm[:, 1:2], sm[:, 0:1], -1.0, None, ALU.mult), reads=["smC"], writes=["smC"])
                op("act", lambda e: e.activation(out=jk64[:, 0:8], in_=lg[:, 0:8], func=AF.Exp, bias=sm[:, 1:2]), reads=["lg", "smC"], writes=["jk64"])
                op("dve", lambda e: e.tensor_reduce(sm[:, 2:3], jk64[:, 0:8], AX.X, ALU.add), reads=["jk64"], writes=["smC"])
                op("dve", lambda e: e.tensor_scalar(gm[:], gm[:], 1e30, -1e30, ALU.mult, ALU.add), reads=["gm"], writes=["gm"])
                op("dve", lambda e: e.tensor_tensor(elm[:], lg[:, 8:72].rearrange("p (a b) -> p a b", a=8), gm[:].unsqueeze(2).to_broadcast([128, 8, 8]), ALU.add),
                   reads=["lg", "gm"], writes=["elm"])
                op("dve", lambda e: e.max(top8[:], elm[:].rearrange("p a b -> p (a b)")), reads=["elm"], writes=["top8"])
                op("dve", lambda e: e.tensor_scalar(A1[:], elm[:].rearrange("p a b -> p (a b)"), top8[:, 0:1], None, ALU.is_equal), reads=["elm", "top8"], writes=["A1"])
                op("dve", lambda e: e.tensor_scalar(A2[:], elm[:].rearrange("p a b -> p (a b)"), top8[:, 1:2], None, ALU.is_equal), reads=["elm", "top8"], writes=["A2"])
                op("dve", lambda e: e.tensor_tensor(Ab[:], A1[:], A2[:], ALU.add), reads=["A1", "A2"], writes=["Ab"])
                op("dve", lambda e: e.tensor_tensor(sm[:, 3:4], top8[:, 1:2], top8[:, 0:1], ALU.subtract), reads=["top8"], writes=["smC"])
                op("act", lambda e: e.activation(out=sm[:, 3:4], in_=sm[:, 3:4], func=AF.Exp), reads=["smC"], writes=["smC"])
                op("dve", lambda e: e.scalar_tensor_tensor(sm[:, 4:5], sm[:, 3:4], 1.0, sm[:, 2:3], ALU.add, ALU.mult), reads=["smC"], writes=["smC"])
                op("dve", lambda e, g=g: e.reciprocal(rt[:, g, 2:3], sm[:, 4:5]), reads=["smC"], writes=["rt%d" % g])
                op("dve", lambda e, g=g: e.tensor_tensor(rt[:, g, 3:4], rt[:, g, 2:3], sm[:, 3:4], ALU.mult), reads=["smC", "rt%d" % g], writes=["rt%d" % g])
                op("pe", lambda e: e.matmul(pl[:, 128:192], ltri[:], Ab[:], start=True, stop=True), reads=["ltri", "Ab"], writes=["plC"])
                op("pe", lambda e: e.matmul(pl[:, 256:320], onesb[:], Ab[:], start=True, stop=True), reads=["onesb", "Ab"], writes=["plC"])
                op("dve", lambda e: e.tensor_tensor(posn[:], pl[:, 128:192], cnt[:], ALU.add), reads=["plC", "cnt"], writes=["posn"])
                op("dve", lambda e: e.tensor_tensor(cnt[:], pl[:, 256:320], cnt[:], ALU.add), reads=["plC", "cnt"], writes=["cnt"])
                op("dve", lambda e: e.tensor_scalar(posn[:], posn[:], float(CAP - 1), None, ALU.min), reads=["posn"], writes=["posn"])
                op("dve", lambda e: e.tensor_tensor(posn[:], posn[:], ecap[:], ALU.add), reads=["posn", "ecap"], writes=["posn"])
                op("dve", lambda e: e.tensor_tensor(jk64[:], A1[:], posn[:], ALU.mult), reads=["A1", "posn"], writes=["jk64"])
                op("dve", lambda e, g=g: e.tensor_reduce(rt[:, g, 0:1], jk64[:], AX.X, ALU.add), reads=["jk64"], writes=["rt%d" % g])
                op("dve", lambda e: e.tensor_tensor(jk64[:], A2[:], posn[:], ALU.mult), reads=["A2", "posn"], writes=["jk64"])
                op("dve", lambda e, g=g: e.tensor_reduce(rt[:, g, 1:2], jk64[:], AX.X, ALU.add), reads=["jk64"], writes=["rt%d" % g])
                op("dve", lambda e, g=g: e.tensor_copy(rti[:, g, :], rt[:, g, 0:2]), reads=["rt%d" % g], writes=["rti%d" % g])
                for k in range(2):
                    dma("pool", "sc%d" % k, lambda e, g=g, k=k: e.indirect_dma_start(out=Xs_d, out_offset=bass.IndirectOffsetOnAxis(ap=rti[:, g, k:k + 1], axis=0),
                                                                                  in_=h3b[:], in_offset=None), reads=[h3bn, "rti%d" % g], writes=["dram_Xs"])

            for t in range(NT + 2):
                A = record(stage2, t - 2) if 2 <= t else []
                B = record(stage1, t - 1) if 1 <= t <= NT else []
                C_ = record(stage0, t) if t < NT else []
                emit_zip(A, B, C_)
            Sx.flush()
        oacc_stack.close()
    aT_stack.close()

    if dbg == "A":
        Sx.stack.close()
        top.close()
        return nc

    with contextlib.ExitStack() as st:
        NSL = 3
        wgu = [alloc(st, "wgu%d" % i, [128, 8, 512], BF16) for i in range(NSL)]
        wd = [alloc(st, "wd%d" % i, [128, 2, D], BF16) for i in range(NSL)]
        xr = [alloc(st, "xr%d" % i, [128, CT, D], BF16) for i in range(NSL)]
        xT = [alloc(st, "xTe%d" % i, [128, 8, CAP], BF16) for i in range(2)]
        sg_ = alloc(st, "sgE", [128, 2, CAP]); hT_ = alloc(st, "hTe", [128, 2, CAP], BF16)
        yo = [alloc(st, "yo%d" % i, [128, D], BF16) for i in range(2)]
        ptrs = [palloc(st, "ptrE%d" % i, [128, 1024], BF16) for i in range(2)]
        pg = [palloc(st, "pg%d" % i, [128, 512]) for i in range(4)]
        pyE = palloc(st, "pyE", [128, 1024])

        def eload(ex):
            sl = ex % NSL
            dma("sp", "wg%d" % sl, lambda e, ex=ex, sl=sl: e.dma_start(out=wgu[sl][:].rearrange("p a b -> p (a b)"), in_=wgub_d[ex]), reads=["dram_wgub"], writes=["wgu%d" % sl])
            dma("sp", "wd%d" % sl, lambda e, ex=ex, sl=sl: e.dma_start(out=wd[sl][:].rearrange("p a b -> p (a b)"), in_=wdb_d[ex]), reads=["dram_wdb"], writes=["wd%d" % sl])
            dma("sp", "lx%d" % sl, lambda e, ex=ex, sl=sl: e.dma_start(out=xr[sl][:], in_=Xs_d[ex * CAP:(ex + 1) * CAP, :].rearrange("(n p) d -> p n d", p=128)),
                reads=["dram_Xs"], writes=["xr%d" % sl])

        def eX(ex):
            sl = ex % NSL
            xt_ = xT[ex % 2]
            for rtile in range(CT):
                ptr = ptrs[rtile % 2]
                for kc in range(8):
                    op("pe", lambda e, kc=kc, rtile=rtile, sl=sl, ptr=ptr: e.transpose(ptr[:, kc * 128:(kc + 1) * 128], xr[sl][:, rtile, kc * 128:(kc + 1) * 128], identb[:]),
                       reads=["xr%d" % sl, "identb"], writes=[rn(ptr)])
                op("act", lambda e, rtile=rtile, ptr=ptr, xt_=xt_: e.activation(out=xt_[:, :, rtile * 128:(rtile + 1) * 128], in_=ptr[:].rearrange("p (a b) -> p a b", a=8), func=AF.Copy),
                   reads=[rn(ptr)], writes=[rn(xt_)])

        def eGU(ex):
            sl = ex % NSL
            xt_ = xT[ex % 2]
            for gu in (1, 0):
                for hc in range(2):
                    for kc in range(8):
                        op("pe", lambda e, gu=gu, hc=hc, kc=kc, sl=sl, xt_=xt_: e.matmul(pg[gu * 2 + hc][:, 0:CAP], wgu[sl][:, kc, gu * 256 + hc * 128:gu * 256 + (hc + 1) * 128], xt_[:, kc, :],
                                                                                       start=(kc == 0), stop=(kc == 7)), reads=["wgu%d" % sl, rn(xt_)], writes=["pg%d" % (gu * 2 + hc)])
            for hc in range(2):
                op("act", lambda e, hc=hc: e.activation(out=sg_[:, hc, :], in_=pg[hc][:, 0:CAP], func=AF.Silu), reads=["pg%d" % hc], writes=["sgE%d" % hc])
                op("dve", lambda e, hc=hc: e.tensor_tensor(hT_[:, hc, :], sg_[:, hc, :], pg[2 + hc][:, 0:CAP], ALU.mult), reads=["sgE%d" % hc, "pg%d" % (2 + hc)], writes=["hTe%d" % hc])

        def eD(ex):
            sl = ex % NSL
            for rtile in range(CT):
                y_ = yo[rtile % 2]
                for hf in range(2):
                    for hc in range(2):
                        op("pe", lambda e, hf=hf, hc=hc, rtile=rtile, sl=sl: e.matmul(pyE[:, hf * 512:(hf + 1) * 512], hT_[:, hc, rtile * 128:(rtile + 1) * 128],
                                                                                  wd[sl][:, hc, hf * 512:(hf + 1) * 512], start=(hc == 0), stop=(hc == 1)),
                           reads=["hTe%d" % hc, "wd%d" % sl], writes=["pyE%d" % hf])
                for hf in range(2):
                    op("act", lambda e, y_=y_, hf=hf: e.activation(out=y_[:, hf * 512:(hf + 1) * 512], in_=pyE[:, hf * 512:(hf + 1) * 512], func=AF.Copy), reads=["pyE%d" % hf], writes=[rn(y_)])
                r0 = ex * CAP + rtile * 128
                dma("act", "sy%d" % (rtile % 2), lambda e, y_=y_, r0=r0: e.dma_start(out=Yb_d[r0:r0 + 128, :], in_=y_[:]), reads=[rn(y_)], writes=["dram_Yb"])

        eload(0); eload(1)
        eX(0); eGU(0)
        for ex in range(NEXP):
            if ex + 2 < NEXP:
                eload(ex + 2)
            if ex + 1 < NEXP:
                eX(ex + 1)
            eD(ex)
            if ex + 1 < NEXP:
                eGU(ex + 1)
        Sx.flush()

    with contextlib.ExitStack() as st:
        x2t = [alloc(st, "x2f%d" % i, [128, D]) for i in range(4)]
        y1 = [alloc(st, "y1f%d" % i, [128, D], BF16) for i in range(4)]
        y2 = [alloc(st, "y2f%d" % i, [128, D], BF16) for i in range(4)]
        def fload(g):
            sl = g % 4
            dma("sp", "fx%d" % sl, lambda e: e.dma_start(out=x2t[sl][:], in_=x2_d[g * 128:(g + 1) * 128, :]), reads=["dram_x2"], writes=["x2f%d" % sl])

        for g in range(min(3, NB * NT)):
            fload(g)
        for g in range(NB * NT):
            sl = g % 4
            if g + 3 < NB * NT:
                fload(g + 3)
            dma("pool", "g1%d" % sl, lambda e, g=g, sl=sl: e.indirect_dma_start(out=y1[sl][:], out_offset=None, in_=Yb_d,
                                                                             in_offset=bass.IndirectOffsetOnAxis(ap=rti[:, g, 0:1], axis=0)),
                reads=["dram_Yb", "rti%d" % g], writes=["y1f%d" % sl])
            dma("pool", "g2%d" % sl, lambda e, g=g, sl=sl: e.indirect_dma_start(out=y2[sl][:], out_offset=None, in_=Yb_d,
                                                                             in_offset=bass.IndirectOffsetOnAxis(ap=rti[:, g, 1:2], axis=0)),
                reads=["dram_Yb", "rti%d" % g], writes=["y2f%d" % sl])
            op("dve", lambda e, g=g, sl=sl: e.scalar_tensor_tensor(x2t[sl][:], y1[sl][:], rt[:, g, 2:3], x2t[sl][:], ALU.mult, ALU.add),
               reads=["y1f%d" % sl, "rt%d" % g, "x2f%d" % sl], writes=["x2f%d" % sl])
            op("dve", lambda e, g=g, sl=sl: e.scalar_tensor_tensor(x2t[sl][:], y2[sl][:], rt[:, g, 3:4], x2t[sl][:], ALU.mult, ALU.add),
               reads=["y2f%d" % sl, "rt%d" % g, "x2f%d" % sl], writes=["x2f%d" % sl])
            dma("act", "fo%d" % sl, lambda e, g=g, sl=sl: e.dma_start(out=y_d[g * 128:(g + 1) * 128, :], in_=x2t[sl][:]), reads=["x2f%d" % sl], writes=["dram_y"])
        Sx.flush()
    Sx.stack.close()
    top.close()
    return nc


def make_in_maps(inputs, S, ncores):
    f = lambda a: np.ascontiguousarray(np.asarray(a))
    half = 8
    inv_freq = (1.0 / (10000.0 ** (np.arange(half, dtype=np.float32) / np.float32(half)))).astype(np.float32)
    half = 16
    inv_freq = (1.0 / (np.float32(10000.0) ** (np.arange(half, dtype=np.float32) / np.float32(half)))).astype(np.float32).reshape(1, 16)
    shared = {
        "inv_freq": inv_freq,
        "norm_mix": f(inputs["norm_mix"][0:1]), "w_in": f(inputs["w_in"][0]),
        "mla_q_a_norm": f(inputs["mla_q_a_norm"][0:1]), "mla_w_q_up": f(inputs["mla_w_q_up"][0]),
        "mla_kv_a_norm": f(inputs["mla_kv_a_norm"][0:1]), "mla_w_kv_up": f(inputs["mla_w_kv_up"][0]),
        "mla_q_norm": f(inputs["mla_q_norm"][0:1]), "mla_k_norm": f(inputs["mla_k_norm"][0:1]),
        "hg_lb_logits": f(np.asarray(inputs["hg_lb_logits"]).reshape(2, 8, 128)), "hg_o_norm": f(inputs["hg_o_norm"][0:1]),
        "w_out": f(inputs["w_out"][0]), "norm_cross": f(inputs["norm_cross"][0:1]), "norm_mem": f(inputs["norm_mem"][0:1]),
        "x_w_q": f(inputs["x_w_q"][0]), "x_w_kv": f(inputs["x_w_kv"][0]),
        "x_q_norm": f(inputs["x_q_norm"][0:1]), "x_k_norm": f(inputs["x_k_norm"][0:1]), "x_w_o": f(inputs["x_w_o"][0]),
        "norm_ffn": f(inputs["norm_ffn"][0:1]),
        "moe_w_router": f(np.concatenate([np.asarray(inputs["moe_w_group"][0]), np.asarray(inputs["moe_w_expert"][0])], axis=1)),
        "moe_b_router": f(np.concatenate([np.asarray(inputs["moe_b_group"][0]), np.asarray(inputs["moe_b_expert"][0])], axis=0).reshape(1, 72)),
        "moe_w_gate": f(inputs["moe_w_gate"][0]), "moe_w_up": f(inputs["moe_w_up"][0]), "moe_w_down": f(inputs["moe_w_down"][0]),
    }
    x = np.asarray(inputs["x"]); mem = np.asarray(inputs["mem"]); pos = np.asarray(inputs["positions"]).astype(np.int32)
    maps = []
    for c in range(ncores):
        m = dict(shared)
        m["x"] = f(x[2 * c:2 * c + 2]); m["mem"] = f(mem[2 * c:2 * c + 2]); m["positions"] = f(pos[2 * c:2 * c + 2])
        maps.append(m)
    return maps


def kernel(**inputs):
    S = 4096
    ncores = 8
    nc = build(S, 384)
    maps = make_in_maps(inputs, S, ncores)
    res = run_bass_kernel_spmd(nc, maps, core_ids=list(range(ncores)))
    out = np.concatenate([np.asarray(r["y"], dtype=np.float32).reshape(2, S, D) for r in res.results], axis=0)
    return out
```

```python
import contextlib
import math
import numpy as np
import concourse.bass as bass
import concourse.mybir as mybir
from concourse.bass_utils import run_bass_kernel_spmd

F32 = mybir.dt.float32
BF16 = mybir.dt.bfloat16
I32 = mybir.dt.int32
U32 = mybir.dt.uint32
AF = mybir.ActivationFunctionType
ALU = mybir.AluOpType
AX = mybir.AxisListType

D = 1024
EPS = 1e-6
NEXP = 64
IN_W = 2912


class Sched:
    ENGS = ("pe", "dve", "act", "pool", "sp")

    def __init__(self, nc, self_sync=False):
        self.nc = nc
        self.self_sync = self_sync
        self.stream = {e: [] for e in self.ENGS}
        self.sem_count = {}
        self.known = {e: {} for e in self.ENGS}
        self.last_w = {}
        self.readers = {}
        self.dma_last = {}
        self.sems = {}
        self.stack = contextlib.ExitStack()
        self.nops = 0

    def _sem(self, name):
        if name not in self.sems:
            self.sems[name] = self.stack.enter_context(self.nc.semaphore(name))
        return self.sems[name]

    def _emit(self, eng, fn, reads, writes, sem, inc, extra=()):
        deps = list(extra)
        for r in reads:
            w = self.last_w.get(r)
            if w is not None:
                deps.append(w)
        for r in writes:
            w = self.last_w.get(r)
            if w is not None:
                deps.append(w)
            deps.extend(self.readers.get(r, ()))
        known = self.known[eng]
        waits = {}
        for (s, v, vc) in deps:
            if s == eng and (eng == "pe" or not self.self_sync):
                continue
            if known.get(s, 0) >= v:
                continue
            waits[s] = max(waits.get(s, 0), v)
            for k2, v2 in vc.items():
                if known.get(k2, 0) < v2:
                    known[k2] = v2
            known[s] = max(known.get(s, 0), v)
        val = self.sem_count.get(sem, 0) + inc
        self.sem_count[sem] = val
        vc = dict(known)
        vc[sem] = val
        me = (sem, val, vc)
        for r in reads:
            self.readers.setdefault(r, []).append(me)
        for r in writes:
            self.last_w[r] = me
            self.readers[r] = []
        self._sem(sem)
        self.stream[eng].append((sorted(waits.items()), fn, sem, inc))
        self.nops += 1
        return me

    def op(self, eng, fn, reads=(), writes=()):
        return self._emit(eng, fn, tuple(reads), tuple(writes), eng, 1)

    def dma(self, queue, key, fn, reads=(), writes=()):
        sem = "d_" + key
        extra = [self.dma_last[sem]] if sem in self.dma_last else []
        me = self._emit(queue, fn, tuple(reads), tuple(writes), sem, 16, extra)
        self.dma_last[sem] = me
        return me

    def flush(self, barrier=True):
        nc = self.nc
        if barrier:
            waits = sorted(self.sem_count.items())
            for e in self.ENGS:
                self.stream[e].append((waits, None, None, 0))
                for s, v in waits:
                    self.known[e][s] = max(self.known[e].get(s, 0), v)
        streams = self.stream
        self.stream = {e: [] for e in self.ENGS}
        sems = self.sems
        with nc.Block() as block:
            def replay(name):
                def body(e):
                    for waits, fn, sem, inc in streams[name]:
                        for s, v in waits:
                            e.wait_ge(sems[s], v)
                        if fn is not None:
                            fn(e).then_inc(sems[sem], inc)
                return body
            block.tensor(replay("pe"))
            block.vector(replay("dve"))
            block.scalar(replay("act"))
            block.gpsimd(replay("pool"))
            block.sync(replay("sp"))


def build(S, CAP, dbg=False):
    NB = 2
    T = NB * S
    NT = S // 128
    QC = min(512, S)
    NQC = S // QC
    BLK = min(512, S)
    CT = CAP // 128
    nc = bass.Bass("TRN2", target_bir_lowering=False)

    def din(name, shape, dt=F32):
        return nc.dram_tensor(name, list(shape), dt, kind="ExternalInput").ap()

    def dscr(name, shape, dt):
        return nc.dram_tensor(name, list(shape), dt, kind=("ExternalOutput" if dbg else "Internal")).ap()

    x_d = din("x", [NB, S, D]); mem_d = din("mem", [NB, 256, D]); pos_d = din("positions", [NB, S], I32)
    invf_d = din("inv_freq", [1, 16])
    norm_mix = din("norm_mix", [1, D]); w_in = din("w_in", [D, IN_W])
    q_a_norm = din("mla_q_a_norm", [1, 192]); w_q_up = din("mla_w_q_up", [192, 768])
    kv_a_norm = din("mla_kv_a_norm", [1, 128]); w_kv_up = din("mla_w_kv_up", [128, 1024])
    q_norm = din("mla_q_norm", [1, 96]); k_norm = din("mla_k_norm", [1, 96])
    lb_logits = din("hg_lb_logits", [2, 8, 128]); o_norm = din("hg_o_norm", [1, 128])
    w_out = din("w_out", [D, D]); norm_cross = din("norm_cross", [1, D]); norm_mem = din("norm_mem", [1, D])
    x_w_q = din("x_w_q", [D, D]); x_w_kv = din("x_w_kv", [D, 2 * D])
    x_q_norm = din("x_q_norm", [1, 256]); x_k_norm = din("x_k_norm", [1, 256]); x_w_o = din("x_w_o", [D, D])
    norm_ffn = din("norm_ffn", [1, D]); w_rt = din("moe_w_router", [D, 72]); b_rt = din("moe_b_router", [1, 72])
    w_gate = din("moe_w_gate", [NEXP, D, 256]); w_up = din("moe_w_up", [NEXP, D, 256]); w_down = din("moe_w_down", [NEXP, 256, D])
    y_d = nc.dram_tensor("y", [T, D], F32, kind="ExternalOutput").ap()

    qT_d = dscr("qT", [NB, 8, 96, S], BF16); kT_d = dscr("kT", [NB, 8, 96, S], BF16)
    vE_d = dscr("vE", [NB, 8, S, 128], BF16); pT_d = dscr("pT", [NB, 12, 128, S], F32)
    hi_d = dscr("hi", [NB, S, 512], BF16); sg_d = dscr("sg", [NB, S, 512], BF16)
    x2_d = dscr("x2", [T, D], F32); h3_d = dscr("h3", [T, D], BF16)
    aT_d = dscr("aTd", [NB, 4, 128, S], BF16)
    Xs_d = dscr("Xs", [NEXP * CAP, D], BF16); Yb_d = dscr("Yb", [NEXP * CAP, D], BF16)
    wgub_d = nc.dram_tensor("wgub", [NEXP, 128, 8 * 512], BF16, kind="Internal").ap()
    wdb_d = nc.dram_tensor("wdb", [NEXP, 128, 2 * D], BF16, kind="Internal").ap()
    wcb_d = nc.dram_tensor("wcb", [3, 128, 8 * D], BF16, kind="Internal").ap()
    winb_d = nc.dram_tensor("winb", [128, 8 * IN_W], BF16, kind="Internal").ap()

    if dbg:
        dx1_d = dscr("dbg_x1", [T, D], F32); dr_d = dscr("dbg_r", [T, 512], BF16)
    Sx = Sched(nc, self_sync=True)
    cur = [Sx]

    def op(*a, **k):
        return cur[0].op(*a, **k)

    def dma(*a, **k):
        return cur[0].dma(*a, **k)

    class Rec:
        def __init__(self):
            self.items = []

        def op(self, *a, **k):
            self.items.append((0, a, k))

        def dma(self, *a, **k):
            self.items.append((1, a, k))

    def record(fn, *args):
        r = Rec()
        cur[0] = r
        try:
            fn(*args)
        finally:
            cur[0] = Sx
        return r.items

    def emit_zip(*lists):
        lists = [l for l in lists if l]
        idx = [0] * len(lists)
        while True:
            best = None
            for q_, l in enumerate(lists):
                if idx[q_] < len(l):
                    pr_ = idx[q_] / len(l)
                    if best is None or pr_ < best[0]:
                        best = (pr_, q_)
            if best is None:
                break
            q_ = best[1]
            it_ = lists[q_][idx[q_]]
            idx[q_] += 1
            (Sx.dma if it_[0] else Sx.op)(*it_[1], **it_[2])
    top = contextlib.ExitStack()

    RN = {}
    uid = [0]

    def alloc(st, name, shape, dt=F32):
        uid[0] += 1
        h = st.enter_context(nc.sbuf_tensor("%s_u%d" % (name, uid[0]), list(shape), dt))
        RN[h.name] = name
        return h

    def palloc(st, name, shape, dt=F32):
        uid[0] += 1
        h = st.enter_context(nc.psum_tensor("%s_u%d" % (name, uid[0]), list(shape), dt))
        RN[h.name] = name
        return h

    def rn(t):
        return RN[t.name]

    def bcast_load(t, vec, n, key):
        dma("sp", key, lambda e: e.dma_start(out=t[:, 0:n], in_=vec.partition_broadcast(128)), writes=[rn(t)])

    identb = alloc(top, "identb", [128, 128], BF16); identf = alloc(top, "identf", [128, 128])
    maskf = alloc(top, "maskf", [128, 128]); maskb = alloc(top, "maskb", [128, 128])
    ltri = alloc(top, "ltri", [128, 128], BF16); onesb = alloc(top, "onesb", [128, 128], BF16)
    g_cross = alloc(top, "g_cross", [128, D]); g_ffn = alloc(top, "g_ffn", [128, D])
    g_o = alloc(top, "g_o", [128, 4, 128]); g_xq = alloc(top, "g_xq", [128, 4, 256])
    b_r = alloc(top, "b_r", [128, 72]); ecap = alloc(top, "ecap", [128, 64])
    lbT = alloc(top, "lbT", [128, 8]); omlT = alloc(top, "omlT", [128, 8]); hoT = alloc(top, "hoT", [128, 8]); lbhT = alloc(top, "lbhT", [128, 8])
    xKT = alloc(top, "xKT", [128, NB, 8, 256], BF16); xV = alloc(top, "xV", [128, NB, 2, D], BF16)
    rt = alloc(top, "rt", [128, NB * NT, 4])
    rti = alloc(top, "rti", [128, NB * NT, 2], I32)
    cnt = alloc(top, "cnt", [128, 64])
    wrt = alloc(top, "wrt", [128, 8, 72])
    tmpc = alloc(top, "tmpc", [128, 256])
    epsb = alloc(top, "epsb", [128, 1])

    with contextlib.ExitStack() as st:
        ptr0 = palloc(st, "ptr0", [128, 512])
        op("pool", lambda e: e.memset(identf[:], 1.0), writes=["identf"])
        op("pool", lambda e: e.affine_select(identf[:], identf[:], [[-1, 128]], ALU.is_equal, 0.0, base=0, channel_multiplier=1),
           reads=["identf"], writes=["identf"])
        op("dve", lambda e: e.tensor_copy(identb[:], identf[:]), reads=["identf"], writes=["identb"])
        op("pool", lambda e: e.memset(maskf[:], 1.0), writes=["maskf"])
        op("pool", lambda e: e.affine_select(maskf[:], maskf[:], [[1, 128]], ALU.is_ge, 0.0, base=0, channel_multiplier=-1),
           reads=["maskf"], writes=["maskf"])
        op("pool", lambda e: e.memset(maskb[:], 1.0), writes=["maskb"])
        op("pool", lambda e: e.affine_select(maskb[:], maskb[:], [[-1, 128]], ALU.is_ge, 0.0, base=0, channel_multiplier=1),
           reads=["maskb"], writes=["maskb"])
        op("pool", lambda e: e.memset(tmpc[:, 0:128], 1.0), writes=["tmpc"])
        op("pool", lambda e: e.affine_select(tmpc[:, 0:128], tmpc[:, 0:128], [[1, 128]], ALU.is_gt, 0.0, base=0, channel_multiplier=-1),
           reads=["tmpc"], writes=["tmpc"])
        op("dve", lambda e: e.tensor_copy(ltri[:], tmpc[:, 0:128]), reads=["tmpc"], writes=["ltri"])
        op("dve", lambda e: e.memset(onesb[:], 1.0), writes=["onesb"])
        op("dve", lambda e: e.memset(epsb[:], EPS), writes=["epsb"])
        op("dve", lambda e: e.memset(cnt[:], 0.0), writes=["cnt"])
        op("pool", lambda e: e.iota(ecap[:], [[CAP, 64]], base=0, channel_multiplier=0, allow_small_or_imprecise_dtypes=True), writes=["ecap"])
        bcast_load(g_cross, norm_cross, D, "c1"); bcast_load(g_ffn, norm_ffn, D, "c2")
        bcast_load(b_r, b_rt, 72, "c1")
        for h in range(4):
            dma("sp", "c0", lambda e, h=h: e.dma_start(out=g_o[:, h, :], in_=o_norm.partition_broadcast(128)), writes=["g_o"])
            dma("sp", "c1", lambda e, h=h: e.dma_start(out=g_xq[:, h, :], in_=x_q_norm.partition_broadcast(128)), writes=["g_xq"])
        op("dve", lambda e: e.tensor_scalar(g_xq[:], g_xq[:], 256.0 ** -0.5, None, ALU.mult), reads=["g_xq"], writes=["g_xq"])
        dma("sp", "c2", lambda e: e.dma_start(out=wrt[:], in_=w_rt.rearrange("(c p) n -> p c n", p=128)), writes=["wrt"])
        dma("sp", "c3", lambda e: e.dma_start(out=tmpc[0:8, 0:128], in_=lb_logits[0]), writes=["tmpc"])
        dma("sp", "c0", lambda e: e.dma_start(out=tmpc[0:8, 128:256], in_=lb_logits[1]), writes=["tmpc"])
        op("dve", lambda e: e.tensor_tensor(tmpc[0:8, 0:128], tmpc[0:8, 0:128], tmpc[0:8, 128:256], ALU.subtract), reads=["tmpc"], writes=["tmpc"])
        op("act", lambda e: e.activation(out=tmpc[0:8, 0:128], in_=tmpc[0:8, 0:128], func=AF.Sigmoid), reads=["tmpc"], writes=["tmpc"])
        op("pe", lambda e: e.transpose(ptr0[:, 0:8], tmpc[0:8, 0:128], identf[0:8, 0:8]), reads=["tmpc", "identf"], writes=["ptr0"])
        op("dve", lambda e: e.tensor_copy(lbT[:], ptr0[:, 0:8]), reads=["ptr0"], writes=["lbT"])
        op("dve", lambda e: e.tensor_scalar(omlT[:], lbT[:], -1.0, 1.0, ALU.mult, ALU.add), reads=["lbT"], writes=["omlT"])
        op("dve", lambda e: e.tensor_scalar(hoT[:], omlT[:], 0.5, None, ALU.mult), reads=["omlT"], writes=["hoT"])
        op("dve", lambda e: e.tensor_tensor(lbhT[:], lbT[:], hoT[:], ALU.add), reads=["lbT", "hoT"], writes=["lbhT"])
        Sx.flush()
    if dbg == "0":
        return nc

    def rstd_from_ms(ms, n, tag):
        op("act", lambda e: e.activation(out=ms[:, 0:n], in_=ms[:, 0:n], func=AF.Ln, bias=epsb[:, 0:1]), reads=[tag, "epsb"], writes=[tag])
        op("act", lambda e: e.activation(out=ms[:, 0:n], in_=ms[:, 0:n], func=AF.Exp, scale=-0.5), reads=[tag], writes=[tag])

    with contextlib.ExitStack() as st:
        wkv = alloc(st, "wkv", [128, 8, 2 * D], BF16)
        g_mem = alloc(st, "g_mem", [128, D]); g_xk = alloc(st, "g_xk", [128, 4, 256])
        mt = alloc(st, "mt", [128, D]); mb = alloc(st, "mb", [128, D], BF16); mT = alloc(st, "mT", [128, 8, 128], BF16)
        junk = alloc(st, "junkM", [128, D]); ms = alloc(st, "msM", [128, 8])
        kf = alloc(st, "kfM", [128, 4, 256]); kb = alloc(st, "kbM", [128, 4, 256], BF16)
        ptr = palloc(st, "ptrM", [128, 1024], BF16)
        pkv = [palloc(st, "pkv%d" % i, [128, 512]) for i in range(4)]
        bcast_load(g_mem, norm_mem, D, "c0")
        for h in range(4):
            dma("sp", "c1", lambda e, h=h: e.dma_start(out=g_xk[:, h, :], in_=x_k_norm.partition_broadcast(128)), writes=["g_xk"])
        for kc in range(8):
            dma("pool", "w%d" % (kc % 2), lambda e, kc=kc: e.dma_start(out=wkv[:, kc, :], in_=x_w_kv[kc * 128:(kc + 1) * 128, :]), writes=["wkv%d" % kc])
        for b in range(NB):
            for mtile in range(2):
                dma("sp", "mx", lambda e, b=b, mtile=mtile: e.dma_start(out=mt[:], in_=mem_d[b, mtile * 128:(mtile + 1) * 128, :]), writes=["mt"])
                op("act", lambda e: e.activation(out=junk[:], in_=mt[:], func=AF.Square, scale=1.0 / 32.0),
                   reads=["mt"], writes=["junkM"])
                op("dve", lambda e: e.tensor_reduce(ms[:, 0:1], junk[:], AX.X, ALU.add), reads=["junkM"], writes=["msM"])
                rstd_from_ms(ms, 1, "msM")
                op("dve", lambda e: e.scalar_tensor_tensor(mb[:], mt[:], ms[:, 0:1], g_mem[:], ALU.mult, ALU.mult), reads=["mt", "msM", "g_mem"], writes=["mb"])
                for kc in range(8):
                    op("pe", lambda e, kc=kc: e.transpose(ptr[:, kc * 128:(kc + 1) * 128], mb[:, kc * 128:(kc + 1) * 128], identb[:]),
                       reads=["mb", "identb"], writes=["ptrM"])
                op("act", lambda e: e.activation(out=mT[:].rearrange("p a b -> p (a b)"), in_=ptr[:], func=AF.Copy), reads=["ptrM"], writes=["mT"])
                for g in range(4):
                    for kc in range(8):
                        op("pe", lambda e, g=g, kc=kc: e.matmul(pkv[g][:], mT[:, kc, :], wkv[:, kc, g * 512:(g + 1) * 512], start=(kc == 0), stop=(kc == 7)),
                           reads=["mT"] + ["wkv%d" % q_ for q_ in range(8)], writes=["pkv%d" % g])
                for g in range(2):
                    op("act", lambda e, g=g: e.activation(out=kf[:, 2 * g:2 * g + 2, :].rearrange("p a b -> p (a b)"), in_=pkv[g][:], func=AF.Copy),
                       reads=["pkv%d" % g], writes=["kfM"])
                    op("dve", lambda e, g=g, b=b, mtile=mtile: e.tensor_copy(xV[:, b, mtile, g * 512:(g + 1) * 512], pkv[2 + g][:]),
                       reads=["pkv%d" % (2 + g)], writes=["xV"])
                for h in range(4):
                    op("act", lambda e, h=h: e.activation(out=junk[:, 0:256], in_=kf[:, h, :], func=AF.Square, scale=1.0 / 16.0),
                       reads=["kfM"], writes=["junkM"])
                    op("dve", lambda e, h=h: e.tensor_reduce(ms[:, h:h + 1], junk[:, 0:256], AX.X, ALU.add), reads=["junkM"], writes=["msM"])
                rstd_from_ms(ms, 4, "msM")
                op("dve", lambda e: e.tensor_tensor(kf[:], kf[:], ms[:, 0:4].unsqueeze(2).to_broadcast([128, 4, 256]), ALU.mult), reads=["kfM", "msM"], writes=["kfM"])
                op("dve", lambda e: e.tensor_tensor(kb[:], kf[:], g_xk[:], ALU.mult), reads=["kfM", "g_xk"], writes=["kbM"])
                for c in range(8):
                    op("pe", lambda e, c=c: e.transpose(ptr[:, c * 128:(c + 1) * 128], kb[:, c // 2, (c % 2) * 128:(c % 2 + 1) * 128], identb[:]),
                       reads=["kbM", "identb"], writes=["ptrM"])
                op("act", lambda e, b=b, mtile=mtile: e.activation(out=xKT[:, b, :, mtile * 128:(mtile + 1) * 128],
                                                                     in_=ptr[:].rearrange("p (a b) -> p a b", a=8), func=AF.Copy),
                   reads=["ptrM"], writes=["xKT"])
        Sx.flush()
    if dbg == "M":
        return nc

    aT_stack = contextlib.ExitStack()
    for b in range(NB):
        with contextlib.ExitStack() as st:
            win = alloc(st, "win", [128, 8, IN_W], BF16)
            g_mix = alloc(st, "g_mix", [128, D]); g_qa = alloc(st, "g_qa", [128, 192]); g_kva = alloc(st, "g_kva", [128, 128])
            gq96 = alloc(st, "gq96", [128, 8, 96]); gk96 = alloc(st, "gk96", [128, 8, 96])
            bcast_load(g_mix, norm_mix, D, "c0"); bcast_load(g_qa, q_a_norm, 192, "c3"); bcast_load(g_kva, kv_a_norm, 128, "c0")
            for h in range(8):
                dma("sp", "c2", lambda e, h=h: e.dma_start(out=gq96[:, h, :], in_=q_norm.partition_broadcast(128)), writes=["gq96"])
                dma("sp", "c3", lambda e, h=h: e.dma_start(out=gk96[:, h, :], in_=k_norm.partition_broadcast(128)), writes=["gk96"])
            op("dve", lambda e: e.tensor_scalar(gq96[:], gq96[:], 96.0 ** -0.5, None, ALU.mult), reads=["gq96"], writes=["gq96"])
            wq = alloc(st, "wq", [128, 2, 768], BF16); wkvu = alloc(st, "wkvu", [128, 1024], BF16)
            cs = alloc(st, "cs", [128, NT, 2, 16]); ang = alloc(st, "ang", [128, NT, 2, 16]); kk = alloc(st, "kk", [128, NT, 2, 16])
            posi = alloc(st, "posi", [NT, 128], I32); posf = alloc(st, "posf", [NT, 128]); posT = alloc(st, "posT", [128, NT])
            invf = alloc(st, "invf", [128, 16])
            xt = [alloc(st, "xt%d" % i, [128, D]) for i in range(2)]
            junk = alloc(st, "junkA", [128, D])
            hb = alloc(st, "hb", [128, D], BF16); hT4s = [alloc(st, "hT4_%d" % i, [128, 8, 512], BF16) for i in range(2)]
            ms = alloc(st, "msA", [128, 4]); msh = alloc(st, "msh", [128, 48])
            cn = alloc(st, "cn", [128, 320], BF16); cnTs = [alloc(st, "cnT%d" % i, [128, 3, 128], BF16) for i in range(2)]; krs = alloc(st, "krs", [128, 32])
            qf = alloc(st, "qf", [128, 8, 96]); kfs = [alloc(st, "kf%d" % i, [128, 8, 96]) for i in range(2)]; sq = alloc(st, "sqA", [128, 8, 96])
            rt4 = alloc(st, "rt4", [128, 4, 8, 16])
            qb = alloc(st, "qb", [128, 8, 96], BF16); kbb = alloc(st, "kbb", [128, 8, 96], BF16)
            qTs = alloc(st, "qTs", [128, 8, 128], BF16); kTs = alloc(st, "kTs", [128, 8, 128], BF16)
            vEt = alloc(st, "vEt", [128, 8, 128], BF16)
            pfs2 = [alloc(st, "pfs%d" % i, [128, 512]) for i in range(2)]; his = alloc(st, "his", [128, 512], BF16); sgs = alloc(st, "sgs", [128, 512], BF16)
            ptr = palloc(st, "ptrA", [128, 1024], BF16)
            ptr1 = palloc(st, "ptrA1", [128, 1024], BF16)
            p1 = palloc(st, "p1", [128, 512]); p2 = palloc(st, "p2", [128, 512])
            pf = [palloc(st, "pf%d" % i, [128, 512]) for i in range(2)]
            qa = palloc(st, "qa", [128, 512]); qb_ = palloc(st, "qb_", [128, 512])
            if b == 0:
                for ci, c0 in enumerate(range(0, IN_W, 512)):
                    c1 = min(IN_W, c0 + 512)
                    dma("pool", "w%d" % (ci % 2), lambda e, c0=c0, c1=c1: e.dma_start(out=win[:, :, c0:c1], in_=w_in.rearrange("(c p) n -> p c n", p=128)[:, :, c0:c1]),
                        writes=["win%d" % ci])
            else:
                dma("sp", "w0", lambda e: e.dma_start(out=win[:].rearrange("p a b -> p (a b)"), in_=winb_d), reads=["dram_winb"], writes=["win%d" % i for i in range(6)])
            dma("pool", "w0", lambda e: e.dma_start(out=wq[:, 0, :], in_=w_q_up[0:128, :]), writes=["wq"])
            op("dve", lambda e: e.memset(wq[:, 1, :], 0.0), writes=["wq"])
            for i_ in range(2):
                op("dve", lambda e, i_=i_: e.memset(cnTs[i_][:, 1, :], 0.0), writes=["cnT%d" % i_])
            dma("pool", "w1", lambda e: e.dma_start(out=wq[0:64, 1, :], in_=w_q_up[128:192, :]), writes=["wq"])
            dma("pool", "w0", lambda e: e.dma_start(out=wkvu[:], in_=w_kv_up), writes=["wkvu"])
            op("dve", lambda e: e.memset(vEt[:], 1.0), writes=["vEt"])
            dma("sp", "c0", lambda e: e.dma_start(out=posi[:], in_=pos_d[b].rearrange("(t p) -> t p", p=128)), writes=["posi"])
            dma("sp", "c1", lambda e: e.dma_start(out=invf[:], in_=invf_d.partition_broadcast(128)), writes=["invf"])
            op("dve", lambda e: e.tensor_copy(posf[:], posi[:]), reads=["posi"], writes=["posf"])
            op("pe", lambda e: e.transpose(p1[:, 0:NT], posf[:], identf[0:NT, 0:NT]), reads=["posf", "identf"], writes=["p1"])
            op("dve", lambda e: e.tensor_copy(posT[:], p1[:, 0:NT]), reads=["p1"], writes=["posT"])
            for tt in range(NT):
                op("dve", lambda e, tt=tt: e.tensor_scalar(ang[:, tt, 0, :], invf[:], posT[:, tt:tt + 1], None, ALU.mult), reads=["invf", "posT"], writes=["ang"])
            op("dve", lambda e: e.tensor_scalar(ang[:, :, 1, :], ang[:, :, 0, :], math.pi / 2, None, ALU.add), reads=["ang"], writes=["ang"])
            MAGIC = 12582912.0
            op("dve", lambda e: e.tensor_scalar(kk[:], ang[:], 1.0 / (2 * math.pi), MAGIC, ALU.mult, ALU.add), reads=["ang"], writes=["kk"])
            op("dve", lambda e: e.tensor_scalar(kk[:], kk[:], MAGIC, None, ALU.subtract), reads=["kk"], writes=["kk"])
            C1 = 6.28125
            C2 = 2 * math.pi - C1
            op("dve", lambda e: e.scalar_tensor_tensor(ang[:].rearrange("p a b c -> p (a b c)"), kk[:].rearrange("p a b c -> p (a b c)"), -C1,
                                                        ang[:].rearrange("p a b c -> p (a b c)"), ALU.mult, ALU.add), reads=["kk", "ang"], writes=["ang"])
            op("dve", lambda e: e.scalar_tensor_tensor(ang[:].rearrange("p a b c -> p (a b c)"), kk[:].rearrange("p a b c -> p (a b c)"), -C2,
                                                        ang[:].rearrange("p a b c -> p (a b c)"), ALU.mult, ALU.add), reads=["kk", "ang"], writes=["ang"])
            op("dve", lambda e: e.tensor_scalar(ang[:], ang[:], 3.14159, -3.14159, ALU.min, ALU.max), reads=["ang"], writes=["ang"])
            op("act", lambda e: e.activation(out=cs[:], in_=ang[:], func=AF.Sin), reads=["ang"], writes=["cs"])

            def qk_finish(src, gain, dstb, dstT, dram, j, tag, dname):
                op("dve", lambda e: e.tensor_tensor(sq[:], src[:], src[:], ALU.mult), reads=[tag], writes=["sqA"])
                op("dve", lambda e: e.tensor_reduce(msh[:, 0:8], sq[:], AX.X, ALU.add), reads=["sqA"], writes=["msh"])
                op("dve", lambda e: e.tensor_scalar(msh[:, 0:8], msh[:, 0:8], 1.0 / 96.0, None, ALU.mult), reads=["msh"], writes=["msh"])
                rstd_from_ms(msh, 8, "msh")
                op("dve", lambda e: e.tensor_tensor(src[:], src[:], msh[:, 0:8].unsqueeze(2).to_broadcast([128, 8, 96]), ALU.mult), reads=[tag, "msh"], writes=[tag])
                op("dve", lambda e: e.tensor_tensor(src[:], src[:], gain[:], ALU.mult), reads=[tag, rn(gain)], writes=[tag])
                sinb = cs[:, j, 0, :].unsqueeze(1).to_broadcast([128, 8, 16])
                cosb = cs[:, j, 1, :].unsqueeze(1).to_broadcast([128, 8, 16])
                op("dve", lambda e: e.tensor_tensor(rt4[:, 0], src[:, :, 64:80], cosb, ALU.mult), reads=[tag, "cs"], writes=["rt4"])
                op("dve", lambda e: e.tensor_tensor(rt4[:, 1], src[:, :, 80:96], sinb, ALU.mult), reads=[tag, "cs"], writes=["rt4"])
                op("dve", lambda e: e.tensor_tensor(rt4[:, 2], src[:, :, 80:96], cosb, ALU.mult), reads=[tag, "cs"], writes=["rt4"])
                op("dve", lambda e: e.tensor_tensor(rt4[:, 3], src[:, :, 64:80], sinb, ALU.mult), reads=[tag, "cs"], writes=["rt4"])
                op("act", lambda e: e.activation(out=dstb[:, :, 0:64], in_=src[:, :, 0:64], func=AF.Copy), reads=[tag], writes=[rn(dstb)])
                op("dve", lambda e: e.tensor_tensor(dstb[:, :, 64:80], rt4[:, 0], rt4[:, 1], ALU.subtract), reads=["rt4"], writes=[rn(dstb)])
                op("dve", lambda e: e.tensor_tensor(dstb[:, :, 80:96], rt4[:, 2], rt4[:, 3], ALU.add), reads=["rt4"], writes=[rn(dstb)])
                for h in range(8):
                    op("pe", lambda e, h=h: e.transpose(ptr1[0:96, h * 128:(h + 1) * 128], dstb[:, h, :], identb[:]), reads=[rn(dstb), "identb"], writes=["ptrA1"])
                op("act", lambda e: e.activation(out=dstT[0:96].rearrange("p a b -> p (a b)"), in_=ptr1[0:96, :], func=AF.Copy), reads=["ptrA1"], writes=[rn(dstT)])
                dma("pool", "st_" + dname, lambda e: e.dma_start(out=dram[b, :, :, j * 128:(j + 1) * 128].rearrange("h d t -> d h t"), in_=dstT[0:96]),
                    reads=[rn(dstT)], writes=["dram_" + dname])

            def loadx(j):
                xs = xt[j % 2]
                dma("sp", "ldx%d" % (j % 2), lambda e, xs=xs, j=j: e.dma_start(out=xs[:], in_=x_d[b, j * 128:(j + 1) * 128, :]), writes=[rn(xs)])

            WIN = ["win%d" % i for i in range(6)]

            def stageA0(j):
                par = j % 2
                xs = xt[par]
                xn = rn(xs)
                cnT = cnTs[par]; cnTn = "cnT%d" % par; kf = kfs[par]; kfn = "kf%d" % par
                gp = (j // 4) % 2; jj = j % 4
                hT4 = hT4s[gp]; hTn = "hT4_%d_%d" % (gp, jj)
                hTv = hT4[:, :, jj * 128:(jj + 1) * 128]
                if j == 0:
                    loadx(0)
                if j + 1 < NT:
                    loadx(j + 1)
                op("act", lambda e: e.activation(out=junk[:], in_=xs[:], func=AF.Square, scale=1.0 / 32.0), reads=[xn], writes=["junkA"])
                op("dve", lambda e: e.tensor_reduce(ms[:, 0:1], junk[:], AX.X, ALU.add), reads=["junkA"], writes=["msA"])
                rstd_from_ms(ms, 1, "msA")
                op("dve", lambda e: e.scalar_tensor_tensor(hb[:], xs[:], ms[:, 0:1], g_mix[:], ALU.mult, ALU.mult), reads=[xn, "msA", "g_mix"], writes=["hb"])
                for kc in range(8):
                    op("pe", lambda e, kc=kc: e.transpose(ptr[:, kc * 128:(kc + 1) * 128], hb[:, kc * 128:(kc + 1) * 128], identb[:]), reads=["hb", "identb"], writes=["ptrA"])
                op("act", lambda e: e.activation(out=hTv, in_=ptr[:].rearrange("p (a b) -> p a b", a=8), func=AF.Copy), reads=["ptrA"], writes=[hTn])
                for kc in range(8):
                    op("pe", lambda e, kc=kc: e.matmul(p1[:, 0:352], hTv[:, kc, :], win[:, kc, 0:352], start=(kc == 0), stop=(kc == 7)), reads=[hTn] + WIN, writes=["p1"])
                for kc in range(8):
                    op("pe", lambda e, kc=kc: e.matmul(p2[:], hTv[:, kc, :], win[:, kc, 1888:2400], start=(kc == 0), stop=(kc == 7)), reads=[hTn] + WIN, writes=["p2"])
                op("act", lambda e: e.activation(out=his[:], in_=p2[:], func=AF.Copy), reads=["p2"], writes=["his"])
                dma("pool", "st_hi", lambda e: e.dma_start(out=hi_d[b, j * 128:(j + 1) * 128, :], in_=his[:]), reads=["his"], writes=["dram_hi"])
                for kc in range(8):
                    op("pe", lambda e, kc=kc: e.matmul(p2[:], hTv[:, kc, :], win[:, kc, 2400:2912], start=(kc == 0), stop=(kc == 7)), reads=[hTn] + WIN, writes=["p2"])
                op("act", lambda e: e.activation(out=sgs[:], in_=p2[:], func=AF.Copy), reads=["p2"], writes=["sgs"])
                dma("pool", "st_sg", lambda e: e.dma_start(out=sg_d[b, j * 128:(j + 1) * 128, :], in_=sgs[:]), reads=["sgs"], writes=["dram_sg"])
                op("act", lambda e: e.activation(out=junk[:, 0:192], in_=p1[:, 0:192], func=AF.Square, scale=192.0 ** -0.5), reads=["p1"], writes=["junkA"])
                op("dve", lambda e: e.tensor_reduce(ms[:, 1:2], junk[:, 0:192], AX.X, ALU.add), reads=["junkA"], writes=["msA"])
                op("act", lambda e: e.activation(out=junk[:, 0:128], in_=p1[:, 192:320], func=AF.Square, scale=128.0 ** -0.5), reads=["p1"], writes=["junkA"])
                op("dve", lambda e: e.tensor_reduce(ms[:, 2:3], junk[:, 0:128], AX.X, ALU.add), reads=["junkA"], writes=["msA"])
                op("act", lambda e: e.activation(out=ms[:, 1:3], in_=ms[:, 1:3], func=AF.Ln, bias=epsb[:, 0:1]), reads=["msA", "epsb"], writes=["msA"])
                op("act", lambda e: e.activation(out=ms[:, 1:3], in_=ms[:, 1:3], func=AF.Exp, scale=-0.5), reads=["msA"], writes=["msA"])
                op("dve", lambda e: e.scalar_tensor_tensor(cn[:, 0:192], p1[:, 0:192], ms[:, 1:2], g_qa[:], ALU.mult, ALU.mult), reads=["p1", "msA", "g_qa"], writes=["cn"])
                op("dve", lambda e: e.scalar_tensor_tensor(cn[:, 192:320], p1[:, 192:320], ms[:, 2:3], g_kva[:], ALU.mult, ALU.mult), reads=["p1", "msA", "g_kva"], writes=["cn"])
                op("dve", lambda e: e.tensor_copy(krs[:], p1[:, 320:352]), reads=["p1"], writes=["krs"])
                op("dve", lambda e: e.tensor_copy(kf[:, :, 64:96], krs[:].unsqueeze(1).to_broadcast([128, 8, 32])), reads=["krs"], writes=[kfn])
                op("pe", lambda e: e.transpose(ptr[:, 0:128], cn[:, 0:128], identb[:]), reads=["cn", "identb"], writes=["ptrA"])
                op("pe", lambda e: e.transpose(ptr[0:64, 128:256], cn[:, 128:192], identb[:]), reads=["cn", "identb"], writes=["ptrA"])
                op("pe", lambda e: e.transpose(ptr[:, 256:384], cn[:, 192:320], identb[:]), reads=["cn", "identb"], writes=["ptrA"])
                op("act", lambda e: e.activation(out=cnT[:, 0, :], in_=ptr[:, 0:128], func=AF.Copy), reads=["ptrA"], writes=[cnTn])
                op("act", lambda e: e.activation(out=cnT[0:64, 1, :], in_=ptr[0:64, 128:256], func=AF.Copy), reads=["ptrA"], writes=[cnTn])
                op("act", lambda e: e.activation(out=cnT[:, 2, :], in_=ptr[:, 256:384], func=AF.Copy), reads=["ptrA"], writes=[cnTn])

            def stageF(G, quarter):
                gp = G % 2
                hT4 = hT4s[gp]
                hTns = ["hT4_%d_%d" % (gp, q_) for q_ in range(4)]
                ntok = min(4, NT - 4 * G) * 128
                for c in range(3 * quarter, 3 * quarter + 3):
                    pfb = pf[c % 2]; pfs_ = pfs2[c % 2]
                    for kc in range(8):
                        op("pe", lambda e, c=c, kc=kc, pfb=pfb: e.matmul(pfb[:, 0:ntok], win[:, kc, 352 + c * 128:352 + (c + 1) * 128], hT4[:, kc, 0:ntok], start=(kc == 0), stop=(kc == 7)),
                           reads=hTns + WIN, writes=[rn(pfb)])
                    op("dve", lambda e, pfb=pfb, pfs_=pfs_: e.tensor_copy(pfs_[:, 0:ntok], pfb[:, 0:ntok]), reads=[rn(pfb)], writes=[rn(pfs_)])
                    dma("pool", "st_pf%d" % (c % 2), lambda e, c=c, pfs_=pfs_: e.dma_start(out=pT_d[b, c, :, G * 512:G * 512 + ntok], in_=pfs_[:, 0:ntok]), reads=[rn(pfs_)], writes=["dram_pT"])

            def stageA1(j):
                par = j % 2
                cnT = cnTs[par]; cnTn = "cnT%d" % par; kf = kfs[par]; kfn = "kf%d" % par
                for g, pt in enumerate((qa, qb_)):
                    op("pe", lambda e, g=g, pt=pt: e.matmul(pt[:, 0:384], cnT[:, 0, :], wq[:, 0, g * 384:(g + 1) * 384], start=True, stop=False), reads=[cnTn, "wq"], writes=[rn(pt)])
                    op("pe", lambda e, g=g, pt=pt: e.matmul(pt[:, 0:384], cnT[:, 1, :], wq[:, 1, g * 384:(g + 1) * 384], start=False, stop=True), reads=[cnTn, "wq"], writes=[rn(pt)])
                    op("act", lambda e, g=g, pt=pt: e.activation(out=qf[:, 4 * g:4 * g + 4, :].rearrange("p a b -> p (a b)"), in_=pt[:, 0:384], func=AF.Copy), reads=[rn(pt)], writes=["qf"])
                for g, pt in enumerate((qa, qb_)):
                    op("pe", lambda e, g=g, pt=pt: e.matmul(pt[:], cnT[:, 2, :], wkvu[:, g * 512:(g + 1) * 512], start=True, stop=True), reads=[cnTn, "wkvu"], writes=[rn(pt)])
                    op("act", lambda e, g=g, pt=pt: e.activation(out=kf[:, 4 * g:4 * g + 4, 0:64], in_=pt[:].rearrange("p (a b) -> p a b", a=4)[:, :, 0:64], func=AF.Copy),
                       reads=[rn(pt)], writes=[kfn])
                    op("act", lambda e, g=g, pt=pt: e.activation(out=vEt[:, 4 * g:4 * g + 4, 0:64], in_=pt[:].rearrange("p (a b) -> p a b", a=4)[:, :, 64:128], func=AF.Copy),
                       reads=[rn(pt)], writes=["vEt"])
                dma("pool", "st_vE", lambda e: e.dma_start(out=vE_d[b, :, j * 128:(j + 1) * 128, :].rearrange("h t c -> t h c"), in_=vEt[:]), reads=["vEt"], writes=["dram_vE"])
                qk_finish(qf, gq96, qb, qTs, qT_d, j, "qf", "qf")
                qk_finish(kf, gk96, kbb, kTs, kT_d, j, kfn, "kf")

            NG = (NT + 3) // 4
            fq = []
            for t in range(NT + 1):
                if t >= 1 and (t % 4 == 0 or t == NT):
                    Gd = (t - 1) // 4
                    fq.extend((Gd, q_) for q_ in range(4))
                A_ = record(stageA1, t - 1) if t >= 1 else []
                B_ = record(stageA0, t) if t < NT else []
                C_ = record(stageF, *fq.pop(0)) if fq else []
                emit_zip(A_, B_, C_)
            while fq:
                emit_zip(record(stageF, *fq.pop(0)))
            Sx.flush()

        if dbg == "A":
            continue
        with contextlib.ExitStack() as st:
            kTh = [alloc(st, "kTh%d" % i, [128, S], BF16) for i in range(2)]
            qTh = [alloc(st, "qTh%d" % i, [128, S], BF16) for i in range(2)]
            vEh = [alloc(st, "vEh%d" % i, [128, NT, 128], BF16) for i in range(2)]
            pTt = [alloc(st, "pTt%d" % i, [128, 2 * QC], BF16) for i in range(3)]
            rc = alloc(st, "rc", [128, QC])
            atmp = [alloc(st, "atmp%d" % i, [128, QC], BF16) for i in range(2)]
            sc = [palloc(st, "sc%d" % i, [128, 1024]) for i in range(3)]
            oa = [palloc(st, "oa%d" % i, [128, 512]) for i in range(2)]
            cgu = [alloc(st, "cgu%d" % i, [128, 8, 512], BF16) for i in range(2)]
            cdn = [alloc(st, "cdn%d" % i, [128, 2, D], BF16) for i in range(2)]
            for ex in range(b * (NEXP // NB), (b + 1) * (NEXP // NB)):
                cs_ = ex % 2
                dma("pool", "cv0", lambda e, ex=ex, cs_=cs_: e.dma_start(out=cgu[cs_][:, :, 0:256], in_=w_gate[ex].rearrange("(c p) h -> p c h", p=128)), writes=["cgu_g%d" % cs_])
                dma("pool", "cv1", lambda e, ex=ex, cs_=cs_: e.dma_start(out=cgu[cs_][:, :, 256:512], in_=w_up[ex].rearrange("(c p) h -> p c h", p=128)), writes=["cgu_u%d" % cs_])
                dma("pool", "cv2", lambda e, ex=ex, cs_=cs_: e.dma_start(out=cdn[cs_][:], in_=w_down[ex].rearrange("(c p) d -> p c d", p=128)), writes=["cdn%d" % cs_])
                dma("pool", "cv3", lambda e, ex=ex, cs_=cs_: e.dma_start(out=wgub_d[ex], in_=cgu[cs_][:].rearrange("p a b -> p (a b)")),
                    reads=["cgu_g%d" % cs_, "cgu_u%d" % cs_], writes=["dram_wgub"])
                dma("pool", "cv4", lambda e, ex=ex, cs_=cs_: e.dma_start(out=wdb_d[ex], in_=cdn[cs_][:].rearrange("p a b -> p (a b)")), reads=["cdn%d" % cs_], writes=["dram_wdb"])
            if b == 0:
                stg = alloc(st, "stgW", [128, 8, D], BF16)
                for wi, wsrc in enumerate((w_out, x_w_q, x_w_o)):
                    dma("pool", "cv5", lambda e, wsrc=wsrc: e.dma_start(out=stg[:], in_=wsrc.rearrange("(c p) n -> p c n", p=128)), writes=["stgW"])
                    dma("pool", "cv6", lambda e, wi=wi: e.dma_start(out=wcb_d[wi], in_=stg[:].rearrange("p a b -> p (a b)")), reads=["stgW"], writes=["dram_wcb"])
                for c0 in range(0, IN_W, 512):
                    c1 = min(IN_W, c0 + 512)
                    dma("pool", "cv5", lambda e, c0=c0, c1=c1: e.dma_start(out=stg[:, :, 0:c1 - c0], in_=w_in.rearrange("(c p) n -> p c n", p=128)[:, :, c0:c1]), writes=["stgW"])
                    dma("pool", "cv6", lambda e, c0=c0, c1=c1: e.dma_start(out=winb_d.rearrange("p (c n) -> p c n", c=8)[:, :, c0:c1], in_=stg[:, :, 0:c1 - c0]), reads=["stgW"], writes=["dram_winb"])
            NP = NT // 2
            steps = [(h, qc, kp) for h in range(8) for qc in range(NQC) for kp in range(NP)]
            nst = len(steps)
            LOOK = 2

            def mla_loads(h):
                sl = h % 2
                dma("sp", "lk%d" % sl, lambda e: e.dma_start(out=kTh[sl][0:96, :], in_=kT_d[b, h]), reads=["dram_kf"], writes=["kTh%d" % sl])
                dma("sp", "lq%d" % sl, lambda e: e.dma_start(out=qTh[sl][0:96, :], in_=qT_d[b, h]), reads=["dram_qf"], writes=["qTh%d" % sl])
                dma("sp", "lv%d" % sl, lambda e: e.dma_start(out=vEh[sl][:], in_=vE_d[b, h].rearrange("(n p) c -> p n c", p=128)), reads=["dram_vE"], writes=["vEh%d" % sl])

            def mla_score(i):
                h, qc, kp = steps[i]
                sl = h % 2
                s_ = sc[i % 3]
                for u in range(2):
                    kt = 2 * kp + u
                    op("pe", lambda e, kt=kt, u=u: e.matmul(s_[:, u * 512:u * 512 + QC], kTh[sl][0:96, kt * 128:(kt + 1) * 128], qTh[sl][0:96, qc * QC:(qc + 1) * QC], start=True, stop=True),
                       reads=["kTh%d" % sl, "qTh%d" % sl], writes=[rn(s_)])

            def mla_exp_pv(i):
                h, qc, kp = steps[i]
                sl = h % 2
                s_ = sc[i % 3]
                p_ = pTt[i % 3]
                o_ = oa[(h * NQC + qc) % 2]
                if QC == 512:
                    op("act", lambda e: e.activation(out=p_[:], in_=s_[:], func=AF.Exp), reads=[rn(s_)], writes=[rn(p_)])
                else:
                    op("act", lambda e: e.activation(out=p_[:].rearrange("p (u q) -> p u q", u=2), in_=s_[:].rearrange("p (u q) -> p u q", u=2)[:, :, 0:QC], func=AF.Exp),
                       reads=[rn(s_)], writes=[rn(p_)])
                for u in range(2):
                    kt = 2 * kp + u
                    op("pe", lambda e, kt=kt, u=u: e.matmul(o_[:, 0:QC], vEh[sl][:, kt, :], p_[:, u * QC:(u + 1) * QC], start=(kt == 0), stop=(kt == NT - 1)),
                       reads=[rn(p_), "vEh%d" % sl], writes=[rn(o_)])
                if kp == NP - 1:
                    op("dve", lambda e: e.reciprocal(rc[0:64, :], o_[64:128, 0:QC]), reads=[rn(o_)], writes=["rc"])
                    tm_ = atmp[(h * NQC + qc) % 2]
                    op("dve", lambda e: e.tensor_tensor(tm_[0:64, :], o_[0:64, 0:QC], rc[0:64, :], ALU.mult), reads=[rn(o_), "rc"], writes=[rn(tm_)])
                    dma("sp", "ash%d" % ((h * NQC + qc) % 2), lambda e: e.dma_start(out=aT_d[b, h // 2, (h % 2) * 64:(h % 2) * 64 + 64, qc * QC:(qc + 1) * QC], in_=tm_[0:64, :]),
                        reads=[rn(tm_)], writes=["dram_aT"])

            mla_loads(0)
            mla_loads(1)
            for i in range(min(LOOK, nst)):
                mla_score(i)
            for i in range(nst):
                h, qc, kp = steps[i]
                if qc == 0 and kp == 0 and 1 <= h and h + 1 < 8:
                    mla_loads(h + 1)
                if i + LOOK < nst:
                    mla_score(i + LOOK)
                mla_exp_pv(i)
            Sx.flush()

        oacc_stack = contextlib.ExitStack()
        oacc = alloc(oacc_stack, "oacc", [128, NT, 512], BF16)
        with contextlib.ExitStack() as st:
            vhs = [alloc(st, "vh%d" % i, [128, NT, 128], BF16) for i in range(2)]
            qc_ = [alloc(st, "qchk%d" % d, [128, S], BF16) for d in range(4)]
            kc_ = [alloc(st, "kchk%d" % d, [128, S], BF16) for d in range(4)]
            eC = [alloc(st, "eC%d" % d, [128, NT]) for d in range(4)]
            zq = alloc(st, "zq", [128, BLK]); thq = alloc(st, "thq", [128, BLK]); sil = alloc(st, "sil", [128, BLK]); t1 = alloc(st, "t1h", [128, BLK])
            zfs = [alloc(st, "zf%d" % d, [128, BLK]) for d in range(2)]; lgfs = [alloc(st, "lgf%d" % d, [128, BLK]) for d in range(2)]
            bbs = [alloc(st, "bb%d" % d, [128, BLK]) for d in range(2)]; wws = [alloc(st, "ww%d" % d, [128, BLK]) for d in range(2)]
            rws = [alloc(st, "rw%d" % d, [128, BLK]) for d in range(2)]
            rmask = alloc(st, "rmask", [128, BLK])
            Sst = [alloc(st, "Sst%d" % d, [128, 128]) for d in range(4)]
            Sdf = [alloc(st, "Sdf%d" % d, [128, 128]) for d in range(4)]
            Sdb = [alloc(st, "Sdb%d" % d, [128, 128], BF16) for d in range(4)]
            ktok = [alloc(st, "ktok%d" % d, [128, 128], BF16) for d in range(4)]
            atm = [alloc(st, "atm%d" % d, [128, 128], BF16) for d in range(4)]
            ptr = palloc(st, "ptrH", [128, 1024], BF16)
            pb = [palloc(st, "pbH%d" % d, [128, 512]) for d in range(4)]
            op("pool", lambda e: e.memset(oacc[:], 0.0), writes=["oacc"])
            SGN = min(NT, 8)
            sgbuf = [alloc(st, "sgbuf%d" % i, [128, SGN, 512], BF16) for i in range(2)]
            for pi in range(NT // SGN):
                sb_ = sgbuf[pi % 2]
                view = sg_d[b, pi * SGN * 128:(pi + 1) * SGN * 128, :].rearrange("(n p) c -> p n c", p=128)
                dma("sp", "sgl%d" % (pi % 2), lambda e, sb_=sb_, view=view: e.dma_start(out=sb_[:], in_=view), reads=["dram_sg"], writes=[rn(sb_)])
                op("act", lambda e, sb_=sb_: e.activation(out=sb_[:], in_=sb_[:], func=AF.Silu), reads=[rn(sb_)], writes=[rn(sb_)])
                dma("sp", "sgs%d" % (pi % 2), lambda e, sb_=sb_, view=view: e.dma_start(out=view, in_=sb_[:]), reads=[rn(sb_)], writes=["dram_sg"])
            op("dve", lambda e: e.memset(rmask[:], 1.0), writes=["rmask"])
            op("dve", lambda e: e.memset(rmask[:].rearrange("p (c t) -> p c t", t=128)[:, :, 0:1], 0.0), writes=["rmask"])
            nbc = BLK // 128
            for hp in range(2):
                for hx in range(2):
                    hh = 2 * hp + hx
                    vh = vhs[hx]
                    dma("sp", "lvh%d" % hx, lambda e, hh=hh, vh=vh: e.dma_start(out=vh[:], in_=hi_d[b, :, hh * 128:(hh + 1) * 128].rearrange("(n p) c -> p n c", p=128)),
                        reads=["dram_hi"], writes=["vh%d" % hx])
                    for blk in range(S // BLK):
                        t0 = blk * BLK
                        dma("sp", "lzq", lambda e, hh=hh, t0=t0: e.dma_start(out=zq[:], in_=pT_d[b, hh, :, t0:t0 + BLK]), reads=["dram_pT"], writes=["zq"])
                        for d in range(2):
                            dma("sp", "lzf%d" % d, lambda e, hh=hh, t0=t0, d=d: e.dma_start(out=zfs[d][:], in_=pT_d[b, 4 + 4 * d + hh, :, t0:t0 + BLK]), reads=["dram_pT"], writes=["zf%d" % d])
                        op("act", lambda e: e.activation(out=thq[:], in_=zq[:], func=AF.Tanh, scale=0.5), reads=["zq"], writes=["thq"])
                        for d in range(2):
                            op("act", lambda e, d=d: e.activation(out=zfs[d][:], in_=zfs[d][:], func=AF.Tanh, scale=0.5), reads=["zf%d" % d], writes=["zf%d" % d])
                        for d in range(2):
                            col = d * 4 + hh
                            op("act", lambda e, d=d, col=col: e.activation(out=lgfs[d][:], in_=zfs[d][:], func=AF.Ln, scale=hoT[:, col:col + 1], bias=lbhT[:, col:col + 1]),
                               reads=["zf%d" % d, "hoT", "lbhT"], writes=["lgf%d" % d])
                        op("dve", lambda e: e.scalar_tensor_tensor(sil[:], thq[:], 1.0, zq[:], ALU.add, ALU.mult), reads=["thq", "zq"], writes=["sil"])
                        for d in range(2):
                            col = d * 4 + hh
                            ch = hx * 2 + d
                            bb = bbs[d]; ww = wws[d]; rw = rws[d]; lgf = lgfs[d]
                            op("dve", lambda e, bb=bb, lgf=lgf: e.tensor_tensor_scan(bb[:], rmask[:], lgf[:], 0.0, ALU.mult, ALU.add), reads=["rmask", "lgf%d" % d], writes=["bb%d" % d])
                            bC = bb[:].rearrange("p (c t) -> p c t", t=128)[:, :, 127:128]
                            op("act", lambda e, ch=ch, blk=blk, bC=bC: e.activation(out=eC[ch][:, blk * nbc:(blk + 1) * nbc].unsqueeze(2), in_=bC, func=AF.Exp),
                               reads=["bb%d" % d], writes=["eC%d" % ch])
                            if d == 0:
                                op("dve", lambda e, bC=bC, bb=bb, ww=ww: e.tensor_tensor(ww[:].rearrange("p (c t) -> p c t", t=128), bC.to_broadcast([128, nbc, 128]),
                                                                                      bb[:].rearrange("p (c t) -> p c t", t=128), ALU.subtract), reads=["bb%d" % d], writes=["ww%d" % d])
                            else:
                                op("dve", lambda e, bb=bb, ww=ww, lgf=lgf: e.tensor_tensor(ww[:], bb[:], lgf[:], ALU.subtract), reads=["bb%d" % d, "lgf%d" % d], writes=["ww%d" % d])
                            op("act", lambda e, ww=ww, rw=rw: e.activation(out=rw[:], in_=ww[:], func=AF.Exp, scale=-1.0), reads=["ww%d" % d], writes=["rw%d" % d])
                            op("act", lambda e, ww=ww: e.activation(out=ww[:], in_=ww[:], func=AF.Exp), reads=["ww%d" % d], writes=["ww%d" % d])
                            op("dve", lambda e, d=d: e.tensor_scalar(t1[:], zfs[d][:], -1.0, 1.0, ALU.mult, ALU.add), reads=["zf%d" % d], writes=["t1h"])
                            op("dve", lambda e, ch=ch, col=col, t0=t0, ww=ww: e.scalar_tensor_tensor(kc_[ch][:, t0:t0 + BLK], t1[:], hoT[:, col:col + 1], ww[:], ALU.mult, ALU.mult),
                               reads=["t1h", "ww%d" % d, "hoT"], writes=["kchk%d" % ch])
                            op("dve", lambda e, ch=ch, t0=t0, rw=rw: e.scalar_tensor_tensor(qc_[ch][:, t0:t0 + BLK], sil[:], 0.5, rw[:], ALU.mult, ALU.mult),
                               reads=["sil", "rw%d" % d], writes=["qchk%d" % ch])
                for ch in range(4):
                    op("dve", lambda e, ch=ch: e.memset(Sst[ch][:], 0.0), writes=["Sst%d" % ch])
                for ci in range(NT):
                    for ch in range(4):
                        hx, d = ch // 2, ch % 2
                        hh = 2 * hp + hx
                        vh = vhs[hx]
                        vhn = "vh%d" % hx
                        c = ci if d == 0 else NT - 1 - ci
                        cs_ = slice(c * 128, (c + 1) * 128)
                        msk = maskf if d == 0 else maskb
                        nm = lambda s, ch=ch: s + str(ch)
                        pat_ = pb[ch][:, 0:128]; po_ = pb[ch][:, 128:256]; pds_ = pb[ch][:, 256:384]
                        op("pe", lambda e, ch=ch, cs_=cs_: e.transpose(ptr[:, ch * 128:(ch + 1) * 128], kc_[ch][:, cs_], identb[:]), reads=[nm("kchk"), "identb"], writes=[nm("ptrH")])
                        op("act", lambda e, ch=ch: e.activation(out=ktok[ch][:], in_=ptr[:, ch * 128:(ch + 1) * 128], func=AF.Copy), reads=[nm("ptrH")], writes=[nm("ktok")])
                        op("pe", lambda e, ch=ch, cs_=cs_, pat_=pat_: e.matmul(pat_, kc_[ch][:, cs_], qc_[ch][:, cs_], start=True, stop=True), reads=[nm("kchk"), nm("qchk")], writes=[nm("pat")])
                        op("dve", lambda e, ch=ch, msk=msk, pat_=pat_: e.tensor_tensor(atm[ch][:], pat_, msk[:], ALU.mult), reads=[nm("pat"), rn(msk)], writes=[nm("atm")])
                        op("dve", lambda e, ch=ch, c=c: e.tensor_scalar(Sdf[ch][:], Sst[ch][:], eC[ch][:, c:c + 1], None, ALU.mult), reads=[nm("Sst"), nm("eC")], writes=[nm("Sdf")])
                        op("act", lambda e, ch=ch, c=c: e.activation(out=Sdb[ch][:], in_=Sst[ch][:], func=AF.Copy, scale=eC[ch][:, c:c + 1]), reads=[nm("Sst"), nm("eC")], writes=[nm("Sdb")])
                        op("pe", lambda e, ch=ch, c=c, vh=vh, po_=po_: e.matmul(po_, atm[ch][:], vh[:, c, :], start=True, stop=False), reads=[nm("atm"), vhn], writes=[nm("poH")])
                        op("pe", lambda e, ch=ch, cs_=cs_, po_=po_: e.matmul(po_, qc_[ch][:, cs_], Sdb[ch][:], start=False, stop=True), reads=[nm("qchk"), nm("Sdb")], writes=[nm("poH")])
                        op("pe", lambda e, ch=ch, c=c, vh=vh, pds_=pds_: e.matmul(pds_, ktok[ch][:], vh[:, c, :], start=True, stop=True), reads=[nm("ktok"), vhn], writes=[nm("pds")])
                        op("dve", lambda e, ch=ch, pds_=pds_: e.tensor_tensor(Sst[ch][:], Sdf[ch][:], pds_, ALU.add), reads=[nm("Sdf"), nm("pds")], writes=[nm("Sst")])
                        oa_ = oacc[:, c, hh * 128:(hh + 1) * 128]
                        oan = "oacc_%d_%d" % (c, hh)
                        op("dve", lambda e, oa_=oa_, po_=po_: e.tensor_tensor(oa_, oa_, po_, ALU.add), reads=[nm("poH"), "oacc", oan], writes=[oan])
            Sx.flush()

        with contextlib.ExitStack() as st:
            wo = alloc(st, "wo", [128, 8, D], BF16); wxq = alloc(st, "wxq", [128, 8, D], BF16); wxo = alloc(st, "wxo", [128, 8, D], BF16)
            xt = [alloc(st, "xtC%d" % i, [128, D]) for i in range(2)]
            sgts = [alloc(st, "sgt%d" % i, [128, 512], BF16) for i in range(2)]; atts = [alloc(st, "att%d" % i, [128, 4, 128], BF16) for i in range(2)]
            junk = alloc(st, "junkC", [128, D], BF16); ms = alloc(st, "msC", [128, 8]); junk1 = alloc(st, "junkC1", [128, D]); ms1 = alloc(st, "msC1", [128, 8])
            of = alloc(st, "of", [128, 4, 128]); rb = alloc(st, "rb", [128, 512], BF16); rT = alloc(st, "rT", [128, 4, 128], BF16)
            x1s = [alloc(st, "x1_%d" % i, [128, D]) for i in range(2)]; h2 = alloc(st, "h2", [128, D], BF16); h2T = alloc(st, "h2T", [128, 8, 128], BF16)
            qx = alloc(st, "qx", [128, 4, 256]); qxb = alloc(st, "qxb", [128, 4, 256], BF16); qxTs = [alloc(st, "qxT_%d" % i, [128, 8, 128], BF16) for i in range(2)]
            pTx = alloc(st, "pTx", [128, 8, 128], BF16); rs = alloc(st, "rs", [128, 4, 128]); oxT = alloc(st, "oxT", [128, 8, 128], BF16)
            x2s = [alloc(st, "x2t_%d" % i, [128, D]) for i in range(2)]; h3f = alloc(st, "h3f", [128, D]); h3bs = [alloc(st, "h3b_%d" % i, [128, D], BF16) for i in range(2)]; h3T = alloc(st, "h3T", [128, 8, 128])
            lg = alloc(st, "lg", [128, 72]); sm = alloc(st, "smC", [128, 16]); gm = alloc(st, "gm", [128, 8]); elm = alloc(st, "elm", [128, 8, 8])
            top8 = alloc(st, "top8", [128, 8]); A1 = alloc(st, "A1", [128, 64]); A2 = alloc(st, "A2", [128, 64]); Ab = alloc(st, "Ab", [128, 64], BF16)
            posn = alloc(st, "posn", [128, 64]); jk64 = alloc(st, "jk64", [128, 64])
            ptr = palloc(st, "ptrC", [128, 1024], BF16)
            py = palloc(st, "py", [128, 1024])
            pss = palloc(st, "pss", [128, 1024])
            psm = palloc(st, "psm", [128, 512])
            pr = palloc(st, "prC", [128, 512])
            pl = palloc(st, "plC", [128, 512])
            dma("sp", "w0", lambda e: e.dma_start(out=wo[:].rearrange("p a b -> p (a b)"), in_=wcb_d[0]), reads=["dram_wcb"], writes=["wo"])
            dma("sp", "w1", lambda e: e.dma_start(out=wxq[:].rearrange("p a b -> p (a b)"), in_=wcb_d[1]), reads=["dram_wcb"], writes=["wxq"])
            dma("sp", "w2", lambda e: e.dma_start(out=wxo[:].rearrange("p a b -> p (a b)"), in_=wcb_d[2]), reads=["dram_wcb"], writes=["wxo"])

            def transpose8(src, dst, dtag, stag):
                for kc in range(8):
                    op("pe", lambda e, kc=kc: e.transpose(ptr[:, kc * 128:(kc + 1) * 128], src[:, kc * 128:(kc + 1) * 128], identb[:]), reads=[stag, "identb"], writes=["ptrC"])
                op("act", lambda e: e.activation(out=dst[:].rearrange("p a b -> p (a b)"), in_=ptr[:], func=AF.Copy), reads=["ptrC"], writes=[dtag])

            def rms_full(src, stag, gain, dst, dtag):
                op("act", lambda e: e.activation(out=junk[:], in_=src[:], func=AF.Square, scale=1.0 / 32.0), reads=[stag], writes=["junkC"])
                op("dve", lambda e: e.tensor_reduce(ms[:, 0:1], junk[:], AX.X, ALU.add), reads=["junkC"], writes=["msC"])
                rstd_from_ms(ms, 1, "msC")
                op("dve", lambda e: e.scalar_tensor_tensor(dst[:], src[:], ms[:, 0:1], gain[:], ALU.mult, ALU.mult), reads=[stag, "msC", rn(gain)], writes=[dtag])

            def cload(j):
                p_ = j % 2
                xs_ = xt[p_]
                dma("sp", "ldx%d" % p_, lambda e: e.dma_start(out=xs_[:], in_=x_d[b, j * 128:(j + 1) * 128, :]), writes=[rn(xs_)])
                dma("sp", "ldat%d" % p_, lambda e: e.dma_start(out=atts[p_][:], in_=aT_d[b, :, :, j * 128:(j + 1) * 128].rearrange("c p t -> p c t")), reads=["dram_aT"], writes=["att%d" % p_])
                dma("sp", "ldsg%d" % p_, lambda e: e.dma_start(out=sgts[p_][:], in_=sg_d[b, j * 128:(j + 1) * 128, :]), reads=["dram_sg"], writes=["sgt%d" % p_])

            def stage0(j):
                g = b * NT + j
                xs = xt[j % 2]
                xn = rn(xs)
                par = j % 2
                x1 = x1s[par]; x1n = "x1_%d" % par; qxT = qxTs[par]; qxTn = "qxT_%d" % par
                att = atts[par]; sgt = sgts[par]; attn = "att%d" % par; sgtn = "sgt%d" % par
                if j == 0:
                    cload(0)
                if j + 1 < NT:
                    cload(j + 1)
                for hh in range(4):
                    op("act", lambda e, hh=hh, j=j: e.activation(out=junk[:, 0:128], in_=oacc[:, j, hh * 128:(hh + 1) * 128], func=AF.Square, scale=128.0 ** -0.5), reads=["oacc"] + ["oacc_%d_%d" % (j, q_) for q_ in range(4)], writes=["junkC"])
                    op("dve", lambda e, hh=hh, j=j: e.tensor_reduce(ms[:, 4 + hh:5 + hh], junk[:, 0:128], AX.X, ALU.add), reads=["junkC"], writes=["msC"])
                op("act", lambda e: e.activation(out=ms[:, 4:8], in_=ms[:, 4:8], func=AF.Ln, bias=epsb[:, 0:1]), reads=["msC", "epsb"], writes=["msC"])
                op("act", lambda e: e.activation(out=ms[:, 4:8], in_=ms[:, 4:8], func=AF.Exp, scale=-0.5), reads=["msC"], writes=["msC"])
                op("dve", lambda e, j=j: e.tensor_tensor(of[:], oacc[:, j, :].rearrange("p (a b) -> p a b", a=4), ms[:, 4:8].unsqueeze(2).to_broadcast([128, 4, 128]), ALU.mult),
                   reads=["oacc", "msC"] + ["oacc_%d_%d" % (j, q_) for q_ in range(4)], writes=["of"])
                op("dve", lambda e: e.tensor_tensor(of[:], of[:], g_o[:], ALU.mult), reads=["of", "g_o"], writes=["of"])
                op("dve", lambda e: e.tensor_tensor(rb[:], of[:].rearrange("p a b -> p (a b)"), sgt[:], ALU.mult), reads=["of", sgtn], writes=["rb"])
                for c in range(4):
                    op("pe", lambda e, c=c: e.transpose(ptr[:, c * 128:(c + 1) * 128], rb[:, c * 128:(c + 1) * 128], identb[:]), reads=["rb", "identb"], writes=["ptrC"])
                op("act", lambda e: e.activation(out=rT[:].rearrange("p a b -> p (a b)"), in_=ptr[:, 0:512], func=AF.Copy), reads=["ptrC"], writes=["rT"])
                for hf in range(2):
                    for c in range(8):
                        lhs = att[:, c, :] if c < 4 else rT[:, c - 4, :]
                        op("pe", lambda e, hf=hf, c=c, lhs=lhs: e.matmul(py[:, hf * 512:(hf + 1) * 512], lhs, wo[:, c, hf * 512:(hf + 1) * 512], start=(c == 0), stop=(c == 7)),
                           reads=[attn, "rT", "wo"], writes=["py"])
                op("dve", lambda e, xs=xs: e.tensor_tensor(x1[:], py[:], xs[:], ALU.add), reads=["py", xn], writes=[x1n])
                if dbg:
                    dma("sp", "dbg1", lambda e, g=g: e.dma_start(out=dx1_d[g * 128:(g + 1) * 128, :], in_=x1[:]), reads=[x1n])
                    dma("sp", "dbg2", lambda e, g=g: e.dma_start(out=dr_d[g * 128:(g + 1) * 128, :], in_=rb[:]), reads=["rb"])
                rms_full(x1, x1n, g_cross, h2, "h2")
                transpose8(h2, h2T, "h2T", "h2")
                for hf in range(2):
                    for c in range(8):
                        op("pe", lambda e, hf=hf, c=c: e.matmul(py[:, hf * 512:(hf + 1) * 512], h2T[:, c, :], wxq[:, c, hf * 512:(hf + 1) * 512], start=(c == 0), stop=(c == 7)),
                           reads=["h2T", "wxq"], writes=["py"])
                op("act", lambda e: e.activation(out=qx[:].rearrange("p a b -> p (a b)"), in_=py[:], func=AF.Copy), reads=["py"], writes=["qx"])
                for hh in range(4):
                    op("act", lambda e, hh=hh: e.activation(out=junk[:, 0:256], in_=qx[:, hh, :], func=AF.Square, scale=1.0 / 16.0),
                       reads=["qx"], writes=["junkC"])
                    op("dve", lambda e, hh=hh: e.tensor_reduce(ms[:, hh:hh + 1], junk[:, 0:256], AX.X, ALU.add), reads=["junkC"], writes=["msC"])
                rstd_from_ms(ms, 4, "msC")
                op("dve", lambda e: e.tensor_tensor(qx[:], qx[:], ms[:, 0:4].unsqueeze(2).to_broadcast([128, 4, 256]), ALU.mult), reads=["qx", "msC"], writes=["qx"])
                op("dve", lambda e: e.tensor_tensor(qxb[:], qx[:], g_xq[:], ALU.mult), reads=["qx", "g_xq"], writes=["qxb"])
                transpose8(qxb[:].rearrange("p a b -> p (a b)") if False else qxb, qxT, "qxT", "qxb") if False else None
                for kc in range(8):
                    op("pe", lambda e, kc=kc: e.transpose(ptr[:, kc * 128:(kc + 1) * 128], qxb[:, kc // 2, (kc % 2) * 128:(kc % 2 + 1) * 128], identb[:]), reads=["qxb", "identb"], writes=["ptrC"])
                op("act", lambda e: e.activation(out=qxT[:].rearrange("p a b -> p (a b)"), in_=ptr[:], func=AF.Copy), reads=["ptrC"], writes=[qxTn])

            def stage1(j):
                g = b * NT + j
                xs = xt[j % 2]
                xn = rn(xs)
                par = j % 2
                x1 = x1s[par]; x1n = "x1_%d" % par; qxT = qxTs[par]; qxTn = "qxT_%d" % par
                x2 = x2s[par]; x2n = "x2t_%d" % par; h3b = h3bs[par]; h3bn = "h3b_%d" % par
                for hh in range(4):
                    for kt in range(2):
                        for dc in range(2):
                            op("pe", lambda e, hh=hh, kt=kt, dc=dc: e.matmul(pss[:, (hh * 2 + kt) * 128:(hh * 2 + kt + 1) * 128], xKT[:, b, hh * 2 + dc, kt * 128:(kt + 1) * 128],
                                                                           qxT[:, hh * 2 + dc, :], start=(dc == 0), stop=(dc == 1)), reads=["xKT", qxTn], writes=["pss"])
                op("act", lambda e: e.activation(out=pTx[:].rearrange("p a b -> p (a b)"), in_=pss[:], func=AF.Exp), reads=["pss"], writes=["pTx"])
                for kt in range(2):
                    op("pe", lambda e, kt=kt: e.matmul(psm[:].rearrange("p (a b) -> p a b", a=4), onesb[:], pTx[:].rearrange("p (h k) t -> p h k t", k=2)[:, :, kt, :],
                                                      start=(kt == 0), stop=(kt == 1)), reads=["pTx", "onesb"], writes=["psm"])
                for hh in range(4):
                    for dc in range(2):
                        for kt in range(2):
                            op("pe", lambda e, hh=hh, kt=kt, dc=dc: e.matmul(pss[:, (hh * 2 + dc) * 128:(hh * 2 + dc + 1) * 128], xV[:, b, kt, hh * 256 + dc * 128:hh * 256 + (dc + 1) * 128],
                                                                           pTx[:, hh * 2 + kt, :], start=(kt == 0), stop=(kt == 1)), reads=["xV", "pTx"], writes=["pss"])
                op("act", lambda e: e.activation(out=rs[:].rearrange("p a b -> p (a b)"), in_=psm[:], func=AF.Ln), reads=["psm"], writes=["rs"])
                op("act", lambda e: e.activation(out=rs[:].rearrange("p a b -> p (a b)"), in_=rs[:].rearrange("p a b -> p (a b)"), func=AF.Exp, scale=-1.0), reads=["rs"], writes=["rs"])
                for dc in range(2):
                    op("dve", lambda e, dc=dc: e.tensor_tensor(oxT[:].rearrange("p (h k) t -> p h k t", k=2)[:, :, dc, :], pss[:].rearrange("p (h k t) -> p h k t", k=2, t=128)[:, :, dc, :],
                                                              rs[:], ALU.mult), reads=["pss", "rs"], writes=["oxT"])
                for hf in range(2):
                    for c in range(8):
                        op("pe", lambda e, hf=hf, c=c: e.matmul(pss[:, hf * 512:(hf + 1) * 512], oxT[:, c, :], wxo[:, c, hf * 512:(hf + 1) * 512], start=(c == 0), stop=(c == 7)),
                           reads=["oxT", "wxo"], writes=["pss"])
                op("dve", lambda e: e.tensor_tensor(x2[:], pss[:], x1[:], ALU.add), reads=["pss", x1n], writes=[x2n])
                dma("pool", "st_x2", lambda e, g=g: e.dma_start(out=x2_d[g * 128:(g + 1) * 128, :], in_=x2[:]), reads=[x2n], writes=["dram_x2"])

            def stage2(j):
                g = b * NT + j
                par = j % 2
                x2 = x2s[par]; x2n = "x2t_%d" % par; h3b = h3bs[par]; h3bn = "h3b_%d" % par
                op("act", lambda e: e.activation(out=junk1[:], in_=x2[:], func=AF.Square, scale=1.0 / 32.0), reads=[x2n], writes=["junkC1"])
                op("dve", lambda e: e.tensor_reduce(ms1[:, 0:1], junk1[:], AX.X, ALU.add), reads=["junkC1"], writes=["msC1"])
                rstd_from_ms(ms1, 1, "msC1")
                op("dve", lambda e: e.scalar_tensor_tensor(h3f[:], x2[:], ms1[:, 0:1], g_ffn[:], ALU.mult, ALU.mult), reads=[x2n, "msC1", "g_ffn"], writes=["h3f"])
                op("act", lambda e: e.activation(out=h3b[:], in_=h3f[:], func=AF.Copy), reads=["h3f"], writes=[h3bn])
                dma("pool", "st_h3", lambda e, g=g: e.dma_start(out=h3_d[g * 128:(g + 1) * 128, :], in_=h3b[:]), reads=[h3bn], writes=["dram_h3"])
                for hf in range(2):
                    for kc in range(4):
                        c = hf * 4 + kc
                        op("pe", lambda e, c=c, kc=kc: e.transpose(pr[:, kc * 128:(kc + 1) * 128], h3f[:, c * 128:(c + 1) * 128], identf[:]), reads=["h3f", "identf"], writes=["prC"])
                    op("act", lambda e, hf=hf: e.activation(out=h3T[:, hf * 4:(hf + 1) * 4, :].rearrange("p a b -> p (a b)"), in_=pr[:], func=AF.Copy), reads=["prC"], writes=["h3T"])
                for c in range(8):
                    op("pe", lambda e, c=c: e.matmul(pl[:, 0:72], h3T[:, c, :], wrt[:, c, :], start=(c == 0), stop=(c == 7)), reads=["h3T", "wrt"], writes=["plC"])
                op("dve", lambda e: e.tensor_tensor(lg[:], pl[:, 0:72], b_r[:], ALU.add), reads=["plC", "b_r"], writes=["lg"])
                op("dve", lambda e: e.tensor_reduce(sm[:, 0:1], lg[:, 0:8], AX.X, ALU.max), reads=["lg"], writes=["smC"])
                op("dve", lambda e: e.tensor_scalar(gm[:], lg[:, 0:8], sm[:, 0:1], None, ALU.is_ge), reads=["lg", "smC"], writes=["gm"])
                op("dve", lambda e: e.tensor_scalar(sm[:, 1:2], sm[:, 0:1], -1.0, None, ALU.mult), reads=["smC"], writes=["smC"])
                op("act", lambda e: e.activation(out=jk64[:, 0:8], in_=lg[:, 0:8], func=AF.Exp, bias=sm[:, 1:2]), reads=["lg", "smC"], writes=["jk64"])
                op("dve", lambda e: e.tensor_reduce(sm[:, 2:3], jk64[:, 0:8], AX.X, ALU.add), reads=["jk64"], writes=["smC"])
                op("dve", lambda e: e.tensor_scalar(gm[:], gm[:], 1e30, -1e30, ALU.mult, ALU.add), reads=["gm"], writes=["gm"])
                op("dve", lambda e: e.tensor_tensor(elm[:], lg[:, 8:72].rearrange("p (a b) -> p a b", a=8), gm[:].unsqueeze(2).to_broadcast([128, 8, 8]), ALU.add),
                   reads=["lg", "gm"], writes=["elm"])
                op("dve", lambda e: e.max(top8[:], elm[:].rearrange("p a b -> p (a b)")), reads=["elm"], writes=["top8"])
                op("dve", lambda e: e.tensor_scalar(A1[:], elm[:].rearrange("p a b -> p (a b)"), top8[:, 0:1], None, ALU.is_equal), reads=["elm", "top8"], writes=["A1"])
                op("dve", lambda e: e.tensor_scalar(A2[:], elm[:].rearrange("p a b -> p (a b)"), top8[:, 1:2], None, ALU.is_equal), reads=["elm", "top8"], writes=["A2"])
                op("dve", lambda e: e.tensor_tensor(Ab[:], A1[:], A2[:], ALU.add), reads=["A1", "A2"], writes=["Ab"])
                op("dve", lambda e: e.tensor_tensor(sm[:, 3:4], top8[:, 1:2], top8[:, 0:1], ALU.subtract), reads=["top8"], writes=["smC"])
                op("act", lambda e: e.activation(out=sm[:, 3:4], in_=sm[:, 3:4], func=AF.Exp), reads=["smC"], writes=["smC"])
                op("dve", lambda e: e.scalar_tensor_tensor(sm[:, 4:5], sm[:, 3:4], 1.0, sm[:, 2:3], ALU.add, ALU.mult), reads=["smC"], writes=["smC"])
                op("dve", lambda e, g=g: e.reciprocal(rt[:, g, 2:3], sm[:, 4:5]), reads=["smC"], writes=["rt%d" % g])
                op("dve", lambda e, g=g: e.tensor_tensor(rt[:, g, 3:4], rt[:, g, 2:3], sm[:, 3:4], ALU.mult), reads=["smC", "rt%d" % g], writes=["rt%d" % g])
                op("pe", lambda e: e.matmul(pl[:, 128:192], ltri[:], Ab[:], start=True, stop=True), reads=["ltri", "Ab"], writes=["plC"])
                op("pe", lambda e: e.matmul(pl[:, 256:320], onesb[:], Ab[:], start=True, stop=True), reads=["onesb", "Ab"], writes=["plC"])
                op("dve", lambda e: e.tensor_tensor(posn[:], pl[:, 128:192], cnt[:], ALU.add), reads=["plC", "cnt"], writes=["posn"])
                op("dve", lambda e: e.tensor_tensor(cnt[:], pl[:, 256:320], cnt[:], ALU.add), reads=["plC", "cnt"], writes=["cnt"])
                op("dve", lambda e: e.tensor_scalar(posn[:], posn[:], float(CAP - 1), None, ALU.min), reads=["posn"], writes=["posn"])
                op("dve", lambda e: e.tensor_tensor(posn[:], posn[:], ecap[:], ALU.add), reads=["posn", "ecap"], writes=["posn"])
                op("dve", lambda e: e.tensor_tensor(jk64[:], A1[:], posn[:], ALU.mult), reads=["A1", "posn"], writes=["jk64"])
                op("dve", lambda e, g=g: e.tensor_reduce(rt[:, g, 0:1], jk64[:], AX.X, ALU.add), reads=["jk64"], writes=["rt%d" % g])
                op("dve", lambda e: e.tensor_tensor(jk64[:], A2[:], posn[:], ALU.mult), reads=["A2", "posn"], writes=["jk64"])
                op("dve", lambda e, g=g: e.tensor_reduce(rt[:, g, 1:2], jk64[:], AX.X, ALU.add), reads=["jk64"], writes=["rt%d" % g])
                op("dve", lambda e, g=g: e.tensor_copy(rti[:, g, :], rt[:, g, 0:2]), reads=["rt%d" % g], writes=["rti%d" % g])
                for k in range(2):
                    dma("pool", "sc%d" % k, lambda e, g=g, k=k: e.indirect_dma_start(out=Xs_d, out_offset=bass.IndirectOffsetOnAxis(ap=rti[:, g, k:k + 1], axis=0),
                                                                                  in_=h3b[:], in_offset=None), reads=[h3bn, "rti%d" % g], writes=["dram_Xs"])

            for t in range(NT + 2):
                A = record(stage2, t - 2) if 2 <= t else []
                B = record(stage1, t - 1) if 1 <= t <= NT else []
                C_ = record(stage0, t) if t < NT else []
                emit_zip(A, B, C_)
            Sx.flush()
        oacc_stack.close()
    aT_stack.close()

    if dbg == "A":
        Sx.stack.close()
        top.close()
        return nc

    with contextlib.ExitStack() as st:
        NSL = 3
        wgu = [alloc(st, "wgu%d" % i, [128, 8, 512], BF16) for i in range(NSL)]
        wd = [alloc(st, "wd%d" % i, [128, 2, D], BF16) for i in range(NSL)]
        xr = [alloc(st, "xr%d" % i, [128, CT, D], BF16) for i in range(NSL)]
        xT = [alloc(st, "xTe%d" % i, [128, 8, CAP], BF16) for i in range(2)]
        sg_ = alloc(st, "sgE", [128, 2, CAP]); hT_ = alloc(st, "hTe", [128, 2, CAP], BF16)
        yo = [alloc(st, "yo%d" % i, [128, D], BF16) for i in range(2)]
        ptrs = [palloc(st, "ptrE%d" % i, [128, 1024], BF16) for i in range(2)]
        pg = [palloc(st, "pg%d" % i, [128, 512]) for i in range(4)]
        pyE = palloc(st, "pyE", [128, 1024])

        def eload(ex):
            sl = ex % NSL
            dma("sp", "wg%d" % sl, lambda e, ex=ex, sl=sl: e.dma_start(out=wgu[sl][:].rearrange("p a b -> p (a b)"), in_=wgub_d[ex]), reads=["dram_wgub"], writes=["wgu%d" % sl])
            dma("sp", "wd%d" % sl, lambda e, ex=ex, sl=sl: e.dma_start(out=wd[sl][:].rearrange("p a b -> p (a b)"), in_=wdb_d[ex]), reads=["dram_wdb"], writes=["wd%d" % sl])
            dma("sp", "lx%d" % sl, lambda e, ex=ex, sl=sl: e.dma_start(out=xr[sl][:], in_=Xs_d[ex * CAP:(ex + 1) * CAP, :].rearrange("(n p) d -> p n d", p=128)),
                reads=["dram_Xs"], writes=["xr%d" % sl])

        def eX(ex):
            sl = ex % NSL
            xt_ = xT[ex % 2]
            for rtile in range(CT):
                ptr = ptrs[rtile % 2]
                for kc in range(8):
                    op("pe", lambda e, kc=kc, rtile=rtile, sl=sl, ptr=ptr: e.transpose(ptr[:, kc * 128:(kc + 1) * 128], xr[sl][:, rtile, kc * 128:(kc + 1) * 128], identb[:]),
                       reads=["xr%d" % sl, "identb"], writes=[rn(ptr)])
                op("act", lambda e, rtile=rtile, ptr=ptr, xt_=xt_: e.activation(out=xt_[:, :, rtile * 128:(rtile + 1) * 128], in_=ptr[:].rearrange("p (a b) -> p a b", a=8), func=AF.Copy),
                   reads=[rn(ptr)], writes=[rn(xt_)])

        def eGU(ex):
            sl = ex % NSL
            xt_ = xT[ex % 2]
            for gu in (1, 0):
                for hc in range(2):
                    for kc in range(8):
                        op("pe", lambda e, gu=gu, hc=hc, kc=kc, sl=sl, xt_=xt_: e.matmul(pg[gu * 2 + hc][:, 0:CAP], wgu[sl][:, kc, gu * 256 + hc * 128:gu * 256 + (hc + 1) * 128], xt_[:, kc, :],
                                                                                       start=(kc == 0), stop=(kc == 7)), reads=["wgu%d" % sl, rn(xt_)], writes=["pg%d" % (gu * 2 + hc)])
            for hc in range(2):
                op("act", lambda e, hc=hc: e.activation(out=sg_[:, hc, :], in_=pg[hc][:, 0:CAP], func=AF.Silu), reads=["pg%d" % hc], writes=["sgE%d" % hc])
                op("dve", lambda e, hc=hc: e.tensor_tensor(hT_[:, hc, :], sg_[:, hc, :], pg[2 + hc][:, 0:CAP], ALU.mult), reads=["sgE%d" % hc, "pg%d" % (2 + hc)], writes=["hTe%d" % hc])

        def eD(ex):
            sl = ex % NSL
            for rtile in range(CT):
                y_ = yo[rtile % 2]
                for hf in range(2):
                    for hc in range(2):
                        op("pe", lambda e, hf=hf, hc=hc, rtile=rtile, sl=sl: e.matmul(pyE[:, hf * 512:(hf + 1) * 512], hT_[:, hc, rtile * 128:(rtile + 1) * 128],
                                                                                  wd[sl][:, hc, hf * 512:(hf + 1) * 512], start=(hc == 0), stop=(hc == 1)),
                           reads=["hTe%d" % hc, "wd%d" % sl], writes=["pyE%d" % hf])
                for hf in range(2):
                    op("act", lambda e, y_=y_, hf=hf: e.activation(out=y_[:, hf * 512:(hf + 1) * 512], in_=pyE[:, hf * 512:(hf + 1) * 512], func=AF.Copy), reads=["pyE%d" % hf], writes=[rn(y_)])
                r0 = ex * CAP + rtile * 128
                dma("act", "sy%d" % (rtile % 2), lambda e, y_=y_, r0=r0: e.dma_start(out=Yb_d[r0:r0 + 128, :], in_=y_[:]), reads=[rn(y_)], writes=["dram_Yb"])

        eload(0); eload(1)
        eX(0); eGU(0)
        for ex in range(NEXP):
            if ex + 2 < NEXP:
                eload(ex + 2)
            if ex + 1 < NEXP:
                eX(ex + 1)
            eD(ex)
            if ex + 1 < NEXP:
                eGU(ex + 1)
        Sx.flush()

    with contextlib.ExitStack() as st:
        x2t = [alloc(st, "x2f%d" % i, [128, D]) for i in range(4)]
        y1 = [alloc(st, "y1f%d" % i, [128, D], BF16) for i in range(4)]
        y2 = [alloc(st, "y2f%d" % i, [128, D], BF16) for i in range(4)]
        def fload(g):
            sl = g % 4
            dma("sp", "fx%d" % sl, lambda e: e.dma_start(out=x2t[sl][:], in_=x2_d[g * 128:(g + 1) * 128, :]), reads=["dram_x2"], writes=["x2f%d" % sl])

        for g in range(min(3, NB * NT)):
            fload(g)
        for g in range(NB * NT):
            sl = g % 4
            if g + 3 < NB * NT:
                fload(g + 3)
            dma("pool", "g1%d" % sl, lambda e, g=g, sl=sl: e.indirect_dma_start(out=y1[sl][:], out_offset=None, in_=Yb_d,
                                                                             in_offset=bass.IndirectOffsetOnAxis(ap=rti[:, g, 0:1], axis=0)),
                reads=["dram_Yb", "rti%d" % g], writes=["y1f%d" % sl])
            dma("pool", "g2%d" % sl, lambda e, g=g, sl=sl: e.indirect_dma_start(out=y2[sl][:], out_offset=None, in_=Yb_d,
                                                                             in_offset=bass.IndirectOffsetOnAxis(ap=rti[:, g, 1:2], axis=0)),
                reads=["dram_Yb", "rti%d" % g], writes=["y2f%d" % sl])
            op("dve", lambda e, g=g, sl=sl: e.scalar_tensor_tensor(x2t[sl][:], y1[sl][:], rt[:, g, 2:3], x2t[sl][:], ALU.mult, ALU.add),
               reads=["y1f%d" % sl, "rt%d" % g, "x2f%d" % sl], writes=["x2f%d" % sl])
            op("dve", lambda e, g=g, sl=sl: e.scalar_tensor_tensor(x2t[sl][:], y2[sl][:], rt[:, g, 3:4], x2t[sl][:], ALU.mult, ALU.add),
               reads=["y2f%d" % sl, "rt%d" % g, "x2f%d" % sl], writes=["x2f%d" % sl])
            dma("act", "fo%d" % sl, lambda e, g=g, sl=sl: e.dma_start(out=y_d[g * 128:(g + 1) * 128, :], in_=x2t[sl][:]), reads=["x2f%d" % sl], writes=["dram_y"])
        Sx.flush()
    Sx.stack.close()
    top.close()
    return nc


def make_in_maps(inputs, S, ncores):
    f = lambda a: np.ascontiguousarray(np.asarray(a))
    half = 8
    inv_freq = (1.0 / (10000.0 ** (np.arange(half, dtype=np.float32) / np.float32(half)))).astype(np.float32)
    half = 16
    inv_freq = (1.0 / (np.float32(10000.0) ** (np.arange(half, dtype=np.float32) / np.float32(half)))).astype(np.float32).reshape(1, 16)
    shared = {
        "inv_freq": inv_freq,
        "norm_mix": f(inputs["norm_mix"][0:1]), "w_in": f(inputs["w_in"][0]),
        "mla_q_a_norm": f(inputs["mla_q_a_norm"][0:1]), "mla_w_q_up": f(inputs["mla_w_q_up"][0]),
        "mla_kv_a_norm": f(inputs["mla_kv_a_norm"][0:1]), "mla_w_kv_up": f(inputs["mla_w_kv_up"][0]),
        "mla_q_norm": f(inputs["mla_q_norm"][0:1]), "mla_k_norm": f(inputs["mla_k_norm"][0:1]),
        "hg_lb_logits": f(np.asarray(inputs["hg_lb_logits"]).reshape(2, 8, 128)), "hg_o_norm": f(inputs["hg_o_norm"][0:1]),
        "w_out": f(inputs["w_out"][0]), "norm_cross": f(inputs["norm_cross"][0:1]), "norm_mem": f(inputs["norm_mem"][0:1]),
        "x_w_q": f(inputs["x_w_q"][0]), "x_w_kv": f(inputs["x_w_kv"][0]),
        "x_q_norm": f(inputs["x_q_norm"][0:1]), "x_k_norm": f(inputs["x_k_norm"][0:1]), "x_w_o": f(inputs["x_w_o"][0]),
        "norm_ffn": f(inputs["norm_ffn"][0:1]),
        "moe_w_router": f(np.concatenate([np.asarray(inputs["moe_w_group"][0]), np.asarray(inputs["moe_w_expert"][0])], axis=1)),
        "moe_b_router": f(np.concatenate([np.asarray(inputs["moe_b_group"][0]), np.asarray(inputs["moe_b_expert"][0])], axis=0).reshape(1, 72)),
        "moe_w_gate": f(inputs["moe_w_gate"][0]), "moe_w_up": f(inputs["moe_w_up"][0]), "moe_w_down": f(inputs["moe_w_down"][0]),
    }
    x = np.asarray(inputs["x"]); mem = np.asarray(inputs["mem"]); pos = np.asarray(inputs["positions"]).astype(np.int32)
    maps = []
    for c in range(ncores):
        m = dict(shared)
        m["x"] = f(x[2 * c:2 * c + 2]); m["mem"] = f(mem[2 * c:2 * c + 2]); m["positions"] = f(pos[2 * c:2 * c + 2])
        maps.append(m)
    return maps


def kernel(**inputs):
    S = 4096
    ncores = 8
    nc = build(S, 384)
    maps = make_in_maps(inputs, S, ncores)
    res = run_bass_kernel_spmd(nc, maps, core_ids=list(range(ncores)))
    out = np.concatenate([np.asarray(r["y"], dtype=np.float32).reshape(2, S, D) for r in res.results], axis=0)
    return out
```
